# Optimizing a Trainium2 kernel written in Bass

```python
import jax, jax.numpy as jnp
from jax import lax
import numpy as np

D_MODEL = 1024
BATCH = 2
SEQ = 16384
DEPTH = 1

GLA_HEADS = 4
GLA_DK = D_MODEL // 2
GLA_DV = D_MODEL
GLA_HEAD_K = GLA_DK // GLA_HEADS
GLA_HEAD_V = GLA_DV // GLA_HEADS
GLA_GATE_RANK = 16
GLA_GATE_TEMP = 16.0
GLA_CHUNK = 64
POOL_WIDTH = D_MODEL // 2
POOL_GROUPS = 4
POOL_GROUP_DIM = POOL_WIDTH // POOL_GROUPS
POOL_WINDOWS = (2, 4, 8, 16)
N_BRANCHES = 2
IN_SPLIT_SIZES = (GLA_DK, GLA_DK, GLA_DV, GLA_DV, GLA_GATE_RANK, POOL_WIDTH, D_MODEL, D_MODEL)
IN_COLS = sum(IN_SPLIT_SIZES)
N_EXPERTS = 64
TOP_K = 8
N_GROUPS = 8
TOP_GROUPS = 4
EXPERTS_PER_GROUP = N_EXPERTS // N_GROUPS
EXPERT_FF = 256
SHARED_FF = 256
ROUTE_SCALE = 2.5
MOE_BLOCK = 128
LN_EPS = 1e-5
RMS_EPS = 1e-6
DEEPNORM_ALPHA = (2.0 * DEPTH) ** 0.25
DEEPNORM_BETA = (8.0 * DEPTH) ** -0.25

kernel_name = "gla_pool_gated_moe_deepnorm_block"


def layer_norm(x, g, b):
    xf = x.astype(jnp.float32)
    mu = xf.mean(-1, keepdims=True)
    var = jnp.square(xf - mu).mean(-1, keepdims=True)
    y = (xf - mu) * lax.rsqrt(var + LN_EPS) * g.astype(jnp.float32) + b.astype(jnp.float32)
    return y.astype(x.dtype)


def rms_norm(x, g):
    xf = x.astype(jnp.float32)
    y = xf * lax.rsqrt(jnp.square(xf).mean(-1, keepdims=True) + RMS_EPS)
    return y * g.astype(jnp.float32)


def gla_chunked(q, k, v, log_g):
    B, S, H, dk = q.shape
    dv = v.shape[-1]
    n_chunks = S // GLA_CHUNK

    def to_chunks(t):
        return t.reshape(B, n_chunks, GLA_CHUNK, H, t.shape[-1]).transpose(1, 0, 3, 2, 4)

    causal = jnp.tril(jnp.ones((GLA_CHUNK, GLA_CHUNK), dtype=bool))[:, :, None]

    def step(state, inp):
        qi, ki, vi, gi = inp
        b = jnp.cumsum(gi, axis=-2)
        diff = b[..., :, None, :] - b[..., None, :, :]
        decay = jnp.exp(jnp.where(causal, diff, -jnp.inf))
        scores = jnp.einsum('bhid,bhjd,bhijd->bhij', qi, ki, decay)
        o_intra = jnp.einsum('bhij,bhjv->bhiv', scores, vi)
        o_inter = jnp.einsum('bhid,bhdv->bhiv', qi * jnp.exp(b), state)
        b_last = b[..., -1:, :]
        k_dec = ki * jnp.exp(b_last - b)
        new_state = (jnp.exp(b_last[..., 0, :])[..., None] * state
                     + jnp.einsum('bhjd,bhjv->bhdv', k_dec, vi))
        return new_state, o_intra + o_inter

    state0 = jnp.zeros((B, H, dk, dv), jnp.float32)
    _, o = lax.scan(step, state0, (to_chunks(q), to_chunks(k), to_chunks(v), to_chunks(log_g)))
    return o.transpose(1, 0, 3, 2, 4).reshape(B, S, H, dv)


def causal_window_mean(u, window):
    S = u.shape[1]
    cs = jnp.cumsum(u.astype(jnp.float32), axis=1)
    shifted = jnp.pad(cs, ((0, 0), (window, 0), (0, 0)))[:, :S]
    count = jnp.minimum(jnp.arange(1, S + 1), window).astype(jnp.float32)
    return (cs - shifted) / count[None, :, None]


def hybrid_mixer(x, w_in, w_gate_up, b_gate, gla_norm_g, w_gla_up,
                 w_pool_grp, pool_scale, w_pool_up, w_out):
    B, S, _ = x.shape
    proj = jnp.einsum('bsd,de->bse', x, w_in)
    split_points = [int(p) for p in np.cumsum(IN_SPLIT_SIZES)[:-1]]
    q, k, v, r, g_lr, u, gate_a, gate_b = jnp.split(proj, split_points, axis=-1)

    log_decay = jax.nn.log_sigmoid(
        jnp.einsum('bsr,rk->bsk', g_lr, w_gate_up).astype(jnp.float32)
        + b_gate.astype(jnp.float32)) / GLA_GATE_TEMP

    def heads(t, d):
        return t.reshape(B, S, GLA_HEADS, d).astype(jnp.float32)

    o = gla_chunked(heads(q, GLA_HEAD_K) * (GLA_HEAD_K ** -0.5), heads(k, GLA_HEAD_K),
                    heads(v, GLA_HEAD_V), log_decay.reshape(B, S, GLA_HEADS, GLA_HEAD_K))
    o = rms_norm(o, gla_norm_g).reshape(B, S, GLA_DV).astype(x.dtype)
    o = o * jax.nn.silu(r)
    y_gla = jnp.einsum('bsv,vd->bsd', o, w_gla_up)

    ug = u.reshape(B, S, POOL_GROUPS, POOL_GROUP_DIM)
    pooled = jnp.stack([causal_window_mean(ug[:, :, i], w) for i, w in enumerate(POOL_WINDOWS)],
                       axis=2) - ug.astype(jnp.float32)
    mixed = jnp.einsum('bsgc,gce->bsge', pooled.astype(x.dtype), w_pool_grp)
    mixed = mixed.reshape(B, S, POOL_WIDTH) * pool_scale
    y_pool = jnp.einsum('bsp,pd->bsd', mixed, w_pool_up)

    merged = jax.nn.sigmoid(gate_a) * y_gla + jax.nn.sigmoid(gate_b) * y_pool
    return jnp.einsum('bsd,de->bse', merged, w_out)


def moe_ffn(x, w_router, router_bias, w_exp_gate, w_exp_up, w_exp_down,
            w_sh_gate, w_sh_up, w_sh_down):
    B, S, D = x.shape
    x2 = x.reshape(B * S, D)
    T = x2.shape[0]
    scores = jax.nn.sigmoid(jnp.einsum('td,de->te', x2, w_router).astype(jnp.float32))
    biased = scores + router_bias.astype(jnp.float32)
    grp = biased.reshape(T, N_GROUPS, EXPERTS_PER_GROUP)
    grp_score = lax.top_k(grp, 2)[0].sum(-1)
    _, top_g = lax.top_k(grp_score, TOP_GROUPS)
    gmask = jax.nn.one_hot(top_g, N_GROUPS, dtype=jnp.float32).sum(-2) > 0
    emask = jnp.repeat(gmask, EXPERTS_PER_GROUP, axis=-1)
    _, top_e = lax.top_k(jnp.where(emask, biased, -jnp.inf), TOP_K)
    sel = jnp.take_along_axis(scores, top_e, axis=-1)
    weights = sel / sel.sum(-1, keepdims=True) * ROUTE_SCALE
    gates = (jax.nn.one_hot(top_e, N_EXPERTS, dtype=jnp.float32) * weights[..., None]).sum(-2)
    gates = gates.astype(x.dtype)

    def expert_block(args):
        xb, gb = args
        h = (jax.nn.silu(jnp.einsum('td,edf->tef', xb, w_exp_gate))
             * jnp.einsum('td,edf->tef', xb, w_exp_up))
        return jnp.einsum('tef,efd->td', h * gb[..., None], w_exp_down)

    n_blk = T // MOE_BLOCK
    routed = lax.map(expert_block, (x2.reshape(n_blk, MOE_BLOCK, D),
                                     gates.reshape(n_blk, MOE_BLOCK, N_EXPERTS)))
    routed = routed.reshape(T, D)
    shared = jnp.einsum('tf,fd->td', jax.nn.silu(x2 @ w_sh_gate) * (x2 @ w_sh_up), w_sh_down)
    return (routed + shared).reshape(B, S, D)


def setup_inputs(seed: int = 0) -> dict:
    key = jax.random.key(seed)
    ks = jax.random.split(key, 24)
    f32 = jnp.float32

    def nrm(k, shape, scale):
        return jax.random.normal(k, shape, f32) * scale

    L = DEPTH
    return {
        "x": nrm(ks[0], (BATCH, SEQ, D_MODEL), 1.0),
        "w_in": nrm(ks[1], (L, D_MODEL, IN_COLS), D_MODEL ** -0.5),
        "w_gate_up": nrm(ks[2], (L, GLA_GATE_RANK, GLA_DK), GLA_GATE_RANK ** -0.5),
        "b_gate": nrm(ks[3], (L, GLA_DK), 0.1),
        "gla_norm_g": 1.0 + nrm(ks[4], (L, GLA_HEAD_V), 0.02),
        "w_gla_up": nrm(ks[5], (L, GLA_DV, D_MODEL), GLA_DV ** -0.5),
        "w_pool_grp": nrm(ks[6], (L, POOL_GROUPS, POOL_GROUP_DIM, POOL_GROUP_DIM), POOL_GROUP_DIM ** -0.5),
        "pool_scale": 1.0 + nrm(ks[7], (L, POOL_WIDTH), 0.02),
        "w_pool_up": nrm(ks[8], (L, POOL_WIDTH, D_MODEL), POOL_WIDTH ** -0.5),
        "w_out": nrm(ks[9], (L, D_MODEL, D_MODEL), D_MODEL ** -0.5) * DEEPNORM_BETA,
        "ln1_g": 1.0 + nrm(ks[10], (L, D_MODEL), 0.02),
        "ln1_b": nrm(ks[11], (L, D_MODEL), 0.02),
        "w_router": nrm(ks[12], (L, D_MODEL, N_EXPERTS), D_MODEL ** -0.5),
        "router_bias": nrm(ks[13], (L, N_EXPERTS), 0.01),
        "w_exp_gate": nrm(ks[14], (L, N_EXPERTS, D_MODEL, EXPERT_FF), D_MODEL ** -0.5),
        "w_exp_up": nrm(ks[15], (L, N_EXPERTS, D_MODEL, EXPERT_FF), D_MODEL ** -0.5),
        "w_exp_down": nrm(ks[16], (L, N_EXPERTS, EXPERT_FF, D_MODEL), EXPERT_FF ** -0.5) * DEEPNORM_BETA,
        "w_sh_gate": nrm(ks[17], (L, D_MODEL, SHARED_FF), D_MODEL ** -0.5),
        "w_sh_up": nrm(ks[18], (L, D_MODEL, SHARED_FF), D_MODEL ** -0.5),
        "w_sh_down": nrm(ks[19], (L, SHARED_FF, D_MODEL), SHARED_FF ** -0.5) * DEEPNORM_BETA,
        "ln2_g": 1.0 + nrm(ks[20], (L, D_MODEL), 0.02),
        "ln2_b": nrm(ks[21], (L, D_MODEL), 0.02),
    }


def reference(x, w_in, w_gate_up, b_gate, gla_norm_g, w_gla_up, w_pool_grp, pool_scale,
              w_pool_up, w_out, ln1_g, ln1_b, w_router, router_bias, w_exp_gate, w_exp_up,
              w_exp_down, w_sh_gate, w_sh_up, w_sh_down, ln2_g, ln2_b):
    h = x
    for layer in range(DEPTH):
        mix = hybrid_mixer(h, w_in[layer], w_gate_up[layer], b_gate[layer], gla_norm_g[layer],
                           w_gla_up[layer], w_pool_grp[layer], pool_scale[layer],
                           w_pool_up[layer], w_out[layer])
        h = layer_norm(DEEPNORM_ALPHA * h + mix, ln1_g[layer], ln1_b[layer])
        ffn = moe_ffn(h, w_router[layer], router_bias[layer], w_exp_gate[layer], w_exp_up[layer],
                      w_exp_down[layer], w_sh_gate[layer], w_sh_up[layer], w_sh_down[layer])
        h = layer_norm(DEEPNORM_ALPHA * h + ffn, ln2_g[layer], ln2_b[layer])
    return h
```

```python
import numpy as np
from contextlib import ExitStack
import concourse.bass as bass
import concourse.mybir as mybir
from concourse.bass_utils import run_bass_kernel_spmd

F32 = mybir.dt.float32
BF16 = mybir.dt.bfloat16
AF = mybir.ActivationFunctionType
ALU = mybir.AluOpType
AX = mybir.AxisListType

D = 1024
QO, KO, VO, RO, GO, UO, GAO, GBO = 0, 512, 1024, 2048, 3072, 3088, 3600, 4624
INC = 5648
NE = 64
ALPHA = float(2.0 ** 0.25)
LN_EPS = 1e-5
RMS_EPS = 1e-6
BLK = 256
TB = 1024
POOL_W = (2, 4, 8, 16)


class Buf:
    __slots__ = ("name", "w", "r", "psum")

    def __init__(self, name, psum=False):
        self.name = name
        self.w = None
        self.r = {}
        self.psum = psum


class Chan:
    def __init__(self, sem):
        self.sem = sem
        self.cnt = 0
        self.pending = []


class Sched:
    def __init__(self, nc, es):
        self.nc = nc
        self.es = es
        self.E = {"pe": nc.tensor, "act": nc.scalar, "dve": nc.vector, "pool": nc.gpsimd, "sp": nc.sync}
        self.sem = {k: es.enter_context(nc.semaphore("c_" + k)) for k in ("pe", "act", "dve", "pool")}
        self.cnt = {k: 0 for k in self.sem}
        self.waited = {k: {} for k in self.E}
        self.chans = []
        self.nwaits = 0
        self.ninstr = 0

    def chan(self, name):
        c = Chan(self.es.enter_context(self.nc.semaphore("d_" + name)))
        self.chans.append(c)
        return c

    def _need(self, e, reads, writes):
        own = self.sem.get(e)
        toks = {}

        def add(t, raw):
            if t is None:
                return
            s, v = t
            if s is own and (e == "pe" or not raw):
                return
            if toks.get(s, 0) < v:
                toks[s] = v
        for b in reads:
            add(b.w, True)
            if b.psum:
                for s, v in b.r.items():
                    add((s, v), False)
        for b in writes:
            add(b.w, False)
            for s, v in b.r.items():
                add((s, v), False)
        wd = self.waited[e]
        for s, v in toks.items():
            if wd.get(s, 0) >= v:
                continue
            self.E[e].wait_ge(s, v)
            wd[s] = v
            self.nwaits += 1

    def prewait(self, e, writes):
        self._need(e, (), writes)

    def _done(self, tok, reads, writes):
        for b in reads:
            b.r[tok[0]] = tok[1]
        for b in writes:
            b.w = tok
            b.r = {}

    def op(self, e, fn, reads=(), writes=()):
        self._need(e, reads, writes)
        ins = fn(self.E[e])
        self.cnt[e] += 1
        ins.then_inc(self.sem[e], 1)
        self.ninstr += 1
        self._done((self.sem[e], self.cnt[e]), reads, writes)

    def mm(self, fns, reads, writes):
        self._need("pe", reads, writes)
        ins = None
        for fn in fns:
            ins = fn(self.nc.tensor)
            self.ninstr += 1
        self.cnt["pe"] += 1
        ins.then_inc(self.sem["pe"], 1)
        self._done((self.sem["pe"], self.cnt["pe"]), reads, writes)

    def dma(self, q, chan, out, in_, reads=(), writes=(), last=True):
        self._need(q, reads, writes)
        ins = self.E[q].dma_start(out=out, in_=in_)
        chan.cnt += 16
        ins.then_inc(chan.sem, 16)
        self.ninstr += 1
        chan.pending.append((tuple(reads), tuple(writes)))
        if last:
            tok = (chan.sem, chan.cnt)
            for r, w in chan.pending:
                self._done(tok, r, w)
            chan.pending = []

    def barrier(self):
        for e in self.E:
            wd = self.waited[e]
            for k, s in self.sem.items():
                if k == e:
                    continue
                if wd.get(s, 0) < self.cnt[k]:
                    self.E[e].wait_ge(s, self.cnt[k])
                    wd[s] = self.cnt[k]
            for c in self.chans:
                if c.cnt and wd.get(c.sem, 0) < c.cnt:
                    self.E[e].wait_ge(c.sem, c.cnt)
                    wd[c.sem] = c.cnt

    def wait_all_on(self, e):
        wd = self.waited[e]
        for k, s in self.sem.items():
            if wd.get(s, 0) < self.cnt[k]:
                self.E[e].wait_ge(s, self.cnt[k])
                wd[s] = self.cnt[k]
        for c in self.chans:
            if c.cnt and wd.get(c.sem, 0) < c.cnt:
                self.E[e].wait_ge(c.sem, c.cnt)
                wd[c.sem] = c.cnt


def build_program(NT, NPREV, dbg=False, stop=None):
    nc = bass.Bass("TRN2", target_bir_lowering=False)
    NBLK = NT // BLK
    NPBLK = NPREV // BLK
    tb = min(TB, NT)
    NTB = NT // tb
    TPB = tb // 128
    NSB = tb // 512

    def din(name, shape):
        return nc.dram_tensor(name, list(shape), F32, kind="ExternalInput").ap()

    xT_d = din("xT", [8, 128, NT])
    x_d = din("x", [NT, D])
    xpT_d = din("xpT", [8, 128, NPREV])
    w_in_d = din("w_in", [D, INC])
    wgu_d = din("wgu17", [32, 512])
    gng_d = din("gng", [128, 2])
    wglaup_d = din("w_gla_up", [D, D])
    wpg_d = din("w_pool_grp", [4, 128, 128])
    pscale_d = din("pscale", [128, 4])
    wpu_d = din("w_pool_up", [512, D])
    wout_d = din("w_out", [D, D])
    ln1g_d = din("ln1g", [128, D])
    ln1b_d = din("ln1b", [128, D])
    ln2g_d = din("ln2g", [128, D])
    ln2b_d = din("ln2b", [128, D])
    wr_d = din("w_router", [D, NE])
    rbias_d = din("rbias", [128, NE])
    weg_d = din("w_eg", [NE + 1, D, 256])
    weu_d = din("w_eu", [NE + 1, D, 256])
    wed_d = din("w_ed", [NE + 1, 256, D])
    triu_d = din("tri_u", [128, 128])
    trir_d = din("tri_r", [128, 128])
    mask4_d = din("mask4", [128, 4, 128])
    ident_d = din("ident", [128, 128])
    ones_d = din("ones", [128, 128])
    bcur_d = din("band_cur", [128, 4, 128])
    bprev_d = din("band_prev", [128, 4, 128])
    bfirst_d = din("band_first", [128, 4, 128])
    out_d = nc.dram_tensor("out", [NT, D], F32, kind="ExternalOutput").ap()
    mT_d = nc.dram_tensor("mT_scr", [128, NT // 128, 8, 128], BF16, kind="Internal").ap()
    h1_d = nc.dram_tensor("h1_scr", [NT, D], F32, kind="Internal").ap()
    dbg_d = {}
    if dbg:
        dbg_d["h1"] = nc.dram_tensor("dbg_h1", [NT, D], F32, kind="ExternalOutput").ap()
        dbg_d["mT"] = nc.dram_tensor("dbg_mT", [128, NT // 128, 8, 128], BF16, kind="ExternalOutput").ap()
        dbg_d["S"] = nc.dram_tensor("dbg_S", [128, 4, 256], F32, kind="ExternalOutput").ap()
        dbg_d["gates"] = nc.dram_tensor("dbg_gates", [128, NT // 128, NE + 1], F32, kind="ExternalOutput").ap()

    with ExitStack() as es0:
        S = Sched(nc, es0)
        psb = [es0.enter_context(nc.psum_tensor("ps%d" % i, [128, 512], F32)) for i in range(8)]
        psbuf = [Buf("ps%d" % i, psum=True) for i in range(8)]
        rr = {"i": 0}

        def ps_next(lo=0, hi=8):
            i = rr.setdefault((lo, hi), lo)
            rr[(lo, hi)] = lo + ((i - lo + 1) % (hi - lo))
            return psb[i], psbuf[i]

        def sbt(es, name, shape, dt):
            return es.enter_context(nc.sbuf_tensor("s_" + name, list(shape), dt))

        triu = sbt(es0, "triu", [128, 128], F32)
        trir = sbt(es0, "trir", [128, 128], F32)
        ident = sbt(es0, "ident", [128, 128], F32)
        onesb = sbt(es0, "onesb", [128, 128], BF16)
        Sst = sbt(es0, "Sst", [128, 4, 256], F32)
        Sbf = sbt(es0, "Sbf", [128, 4, 256], BF16)
        c_init = S.chan("init")
        c_initp = S.chan("initp")
        c_dbg = [S.chan("dbg%d" % i) for i in range(4)] if dbg else None
        B_const = Buf("const")
        B_csp = Buf("const_sp")
        B_cpool = Buf("const_pool")
        B_S = Buf("S")
        B_Sbf = Buf("Sbf")
        S.dma("sp", c_init, triu[:], triu_d, writes=[B_csp], last=False)
        S.dma("sp", c_init, trir[:], trir_d, writes=[B_csp], last=False)
        S.dma("sp", c_init, ident[:], ident_d, writes=[B_csp], last=False)
        S.dma("pool", c_initp, onesb[:], ones_d, writes=[B_cpool], last=False)
        S.op("dve", lambda e: e.memset(Sst[:], 0.0), writes=[B_S])
        S.op("dve", lambda e: e.memset(Sbf[:], 0.0), writes=[B_Sbf])

        with ExitStack() as esA:
            w_in = sbt(esA, "w_in", [128, 8, INC], BF16)
            wgu = sbt(esA, "wgu", [32, 512], BF16)
            wglaup = sbt(esA, "wglaup", [128, 8, D], BF16)
            wpg = sbt(esA, "wpg", [128, 4, 128], BF16)
            wpu = sbt(esA, "wpu", [128, 4, D], BF16)
            gng = sbt(esA, "gng", [128, 2], F32)
            g16 = sbt(esA, "g16", [128, 2], F32)
            pscale = sbt(esA, "pscale", [128, 4], F32)
            mask4 = sbt(esA, "mask4", [128, 4, 128], BF16)
            bcur = sbt(esA, "bcur", [128, 4, 128], BF16)
            bprev = sbt(esA, "bprev", [128, 4, 128], BF16)
            bfirst = sbt(esA, "bfirst", [128, 4, 128], BF16)
            w_in_v = w_in_d.rearrange("(c p) n -> p c n", p=128)
            S.dma("pool", c_initp, wgu[:], wgu_d, writes=[B_cpool], last=False)
            for (lo, hi) in ((GO, GO + 16), (KO, KO + 512), (VO, VO + 1024), (UO, UO + 512)):
                S.dma("pool", c_initp, w_in[:, :, lo:hi], w_in_v[:, :, lo:hi], writes=[B_cpool], last=False)
            S.dma("sp", c_init, gng[:], gng_d, writes=[B_csp], last=False)
            S.dma("sp", c_init, pscale[:], pscale_d, writes=[B_csp], last=True)
            S.dma("pool", c_initp, mask4[:], mask4_d, writes=[B_cpool], last=False)
            S.dma("pool", c_initp, bcur[:], bcur_d, writes=[B_cpool], last=False)
            S.dma("pool", c_initp, bprev[:], bprev_d, writes=[B_cpool], last=False)
            S.dma("pool", c_initp, bfirst[:], bfirst_d, writes=[B_cpool], last=True)
            c_init2 = S.chan("init2")
            B_const2 = Buf("const2")
            for (lo, hi) in ((QO, QO + 512), (RO, RO + 1024), (GAO, GAO + 1024), (GBO, GBO + 1024)):
                S.dma("pool", c_init2, w_in[:, :, lo:hi], w_in_v[:, :, lo:hi], writes=[B_const2], last=False)
            S.dma("pool", c_init2, wglaup[:], wglaup_d.rearrange("(c p) n -> p c n", p=128), writes=[B_const2], last=False)
            S.dma("pool", c_init2, wpg[:], wpg_d.rearrange("g c e -> c g e"), writes=[B_const2], last=False)
            S.dma("pool", c_init2, wpu[:], wpu_d.rearrange("(c p) n -> p c n", p=128), writes=[B_const2], last=True)
            S.op("dve", lambda e: e.tensor_scalar_mul(out=g16[:], in0=gng[:], scalar1=16.0), reads=[B_csp, B_cpool], writes=[B_const])

            if stop == 'init':
                S.barrier()
                return nc
            NXS = 2
            xTb = [sbt(esA, "xTb%d" % i, [128, 8, BLK], BF16) for i in range(NXS)]
            B_xTb = [Buf("xTb%d" % i) for i in range(NXS)]
            c_x = [S.chan("x%d" % i) for i in range(NXS)]
            glr = [sbt(esA, "glr%d" % i, [32, BLK], BF16) for i in range(2)]
            B_glr = [Buf("glr%d" % i) for i in range(2)]
            for i in range(2):
                S.op("dve", (lambda i: lambda e: e.memset(glr[i][:], 1.0))(i), writes=[B_glr[i]])
            e1 = sbt(esA, "e1", [128, 512], F32)
            B_e1 = Buf("e1")
            sp = [sbt(esA, "sp%d" % i, [128, 512], F32) for i in range(1)] * 2
            B_sp = [Buf("sp%d" % i) for i in range(1)] * 2
            ebT = [sbt(esA, "ebT%d" % i, [128, 4, BLK], F32) for i in range(1)] * 2
            B_ebT = [Buf("ebT%d" % i) for i in range(1)] * 2
            enbT = [sbt(esA, "enbT%d" % i, [128, 4, BLK], F32) for i in range(1)] * 2
            B_enbT = [Buf("enbT%d" % i) for i in range(1)] * 2
            edl = [sbt(esA, "edl%d" % i, [128, 4], F32) for i in range(2)]
            B_edl = [Buf("edl%d" % i) for i in range(2)]
            erb = [sbt(esA, "erb%d" % i, [128, 512], F32) for i in range(1)] * 2
            B_erb = [Buf("erb%d" % i) for i in range(1)] * 2
            kdec = [sbt(esA, "kdec%d" % i, [128, 512], BF16) for i in range(2)]
            B_kdec = [Buf("kdec%d" % i) for i in range(2)]
            vsb = [sbt(esA, "vsb%d" % i, [128, 1024], BF16) for i in range(2)]
            B_vsb = [Buf("vsb%d" % i) for i in range(2)]
            usb = [sbt(esA, "usb%d" % i, [128, 512], BF16) for i in range(3)]
            B_usb = [Buf("usb%d" % i) for i in range(3)]
            qtT = [sbt(esA, "qtT%d" % i, [128, 4, BLK], BF16) for i in range(1)] * 2
            B_qtT = [Buf("qtT%d" % i) for i in range(1)] * 2
            ktT = [sbt(esA, "ktT%d" % i, [128, 4, BLK], BF16) for i in range(1)] * 2
            B_ktT = [Buf("ktT%d" % i) for i in range(1)] * 2
            srT = [sbt(esA, "srT%d" % i, [128, 8, BLK], BF16) for i in range(1)] * 2
            B_srT = [Buf("srT%d" % i) for i in range(1)] * 2
            sga = [sbt(esA, "sga%d" % i, [128, BLK], BF16) for i in range(2)]
            B_sga = [Buf("sga%d" % i) for i in range(2)]
            sgb = [sbt(esA, "sgb%d" % i, [128, BLK], BF16) for i in range(2)]
            B_sgb = [Buf("sgb%d" % i) for i in range(2)]
            AT = [sbt(esA, "AT%d" % i, [128, 4, 128], BF16) for i in range(2)]
            B_AT = [Buf("AT%d" % i) for i in range(2)]
            sq = [sbt(esA, "sq%d" % i, [128, 8, 128], BF16) for i in range(1)] * 2
            B_sq = [Buf("sq%d" % i) for i in range(1)] * 2
            rstd = [sbt(esA, "rstd%d" % i, [128, 4, 128], F32) for i in range(1)] * 2
            B_rstd = [Buf("rstd%d" % i) for i in range(1)] * 2
            otmp = [sbt(esA, "otmp%d" % i, [128, 8, 128], F32) for i in range(1)] * 2
            B_otmp = [Buf("otmp%d" % i) for i in range(1)] * 2
            ogT = [sbt(esA, "ogT%d" % i, [128, 8, BLK], BF16) for i in range(1)] * 2
            B_ogT = [Buf("ogT%d" % i) for i in range(1)] * 2
            plT = [sbt(esA, "plT%d" % i, [128, 4, 128], BF16) for i in range(2)]
            B_plT = [Buf("plT%d" % i) for i in range(2)]
            mxT = [sbt(esA, "mxT%d" % i, [128, 4, BLK], BF16) for i in range(2)]
            B_mxT = [Buf("mxT%d" % i) for i in range(2)]
            t1 = [sbt(esA, "t1_%d" % i, [128, BLK], F32) for i in range(1)] * 2
            B_t1 = [Buf("t1_%d" % i) for i in range(1)] * 2
            t2 = [sbt(esA, "t2_%d" % i, [128, BLK], F32) for i in range(1)] * 2
            B_t2 = [Buf("t2_%d" % i) for i in range(1)] * 2
            mgT = [sbt(esA, "mgT%d" % i, [128, 2, 8, 128], BF16) for i in range(1)] * 2
            B_mgT = [Buf("mgT%d" % i) for i in range(1)] * 2
            c_mg = [S.chan("mg0")] * 2
            B_mTd = Buf("mT_dram")

            st = {"tile": 0, "u": 0}
            pend = {"f": None}

            def load_x(src, blk, slot):
                S.dma("pool", c_x[slot], xTb[slot][:], src[:, :, blk * BLK:(blk + 1) * BLK].rearrange("c p t -> p c t"),
                      writes=[B_xTb[slot]])

            def proj_fm(slot, col0, M, n0, n1):
                pt, pb = ps_next()
                xs = xTb[slot]
                S.mm([(lambda c: lambda pe: pe.matmul(pt[0:M, 0:n1 - n0], lhsT=w_in[:, c, col0:col0 + M],
                                                       rhs=xs[:, c, n0:n1], start=(c == 0), stop=(c == 7)))(c)
                      for c in range(8)], reads=[B_xTb[slot], B_const, B_const2], writes=[pb])
                return pt, pb

            def proj_tm(slot, col0, N, tl):
                pt, pb = ps_next()
                xs = xTb[slot]
                S.mm([(lambda c: lambda pe: pe.matmul(pt[:, 0:N], lhsT=xs[:, c, tl * 128:(tl + 1) * 128],
                                                       rhs=w_in[:, c, col0:col0 + N], start=(c == 0), stop=(c == 7)))(c)
                      for c in range(8)], reads=[B_xTb[slot], B_const, B_const2], writes=[pb])
                return pt, pb

            def gla_block(src, blk, slot, main, want_u, first_main_blk):
                par = blk % 2 if main else (blk % 2)
                b2 = blk % 2
                pt, pb = proj_fm(slot, GO, 16, 0, BLK)
                S.op("act", lambda e: e.copy(out=glr[b2][0:16, :], in_=pt[0:16, 0:BLK]), reads=[pb], writes=[B_glr[b2]])
                for tl in range(BLK // 128):
                    ti = st["tile"]
                    st["tile"] += 1
                    p2 = ti % 2
                    tsl = slice(tl * 128, (tl + 1) * 128)
                    pz, bz = ps_next()
                    S.mm([lambda pe: pe.matmul(pz[:, :], lhsT=glr[b2][0:17, tsl], rhs=wgu[0:17, :], start=True, stop=True)],
                         reads=[B_glr[b2], B_const], writes=[bz])
                    S.op("act", lambda e: e.activation(out=e1[:], in_=pz[:, :], func=AF.Exp, scale=-1.0), reads=[bz], writes=[B_e1])
                    S.op("act", lambda e: e.activation(out=sp[p2][:], in_=e1[:], func=AF.Ln, bias=1.0), reads=[B_e1], writes=[B_sp[p2]])
                    pk, bk = proj_tm(slot, KO, 512, tl)
                    pv0, bv0 = proj_tm(slot, VO, 512, tl)
                    pv1, bv1 = proj_tm(slot, VO + 512, 512, tl)
                    S.op("act", lambda e: e.copy(out=vsb[p2][:, 0:512], in_=pv0[:, :]), reads=[bv0], writes=[B_vsb[p2]])
                    S.op("dve", lambda e: e.tensor_copy(out=vsb[p2][:, 512:1024], in_=pv1[:, :]), reads=[bv1], writes=[B_vsb[p2]])
                    if want_u:
                        st["u"] = (st["u"] + 1) % 3
                        ui = st["u"]
                        pu, bu = proj_tm(slot, UO, 512, tl)
                        S.op("act", lambda e: e.copy(out=usb[ui][:], in_=pu[:, :]), reads=[bu], writes=[B_usb[ui]])
                    prb, brb = ps_next()
                    S.mm([lambda pe: pe.matmul(prb[:, :], lhsT=trir[:, :], rhs=sp[p2][:, :], start=True, stop=True)],
                         reads=[B_sp[p2], B_const], writes=[brb])
                    S.op("act", lambda e: e.activation(out=erb[p2][:], in_=prb[:, :], func=AF.Exp), reads=[brb], writes=[B_erb[p2]])
                    S.op("dve", lambda e: e.tensor_tensor(out=kdec[p2][:], in0=pk[:, :], in1=erb[p2][:], op=ALU.mult),
                         reads=[bk, B_erb[p2]], writes=[B_kdec[p2]])
                    pbT, bbT = ps_next()
                    if main:
                        for h in range(4):
                            S.mm([(lambda h: lambda pe: pe.matmul(pbT[:, h * 128:(h + 1) * 128], lhsT=sp[p2][:, h * 128:(h + 1) * 128],
                                                                   rhs=triu[:, :], start=True, stop=True))(h)],
                                 reads=[B_sp[p2], B_const], writes=[bbT])
                        pb3 = pbT[:, :].rearrange("p (h i) -> p h i", h=4)
                        S.op("act", lambda e: e.activation(out=ebT[b2][:, :, tsl], in_=pb3, func=AF.Exp), reads=[bbT], writes=[B_ebT[b2]])
                        S.op("act", lambda e: e.activation(out=enbT[b2][:, :, tsl], in_=pb3, func=AF.Exp, scale=-1.0), reads=[bbT], writes=[B_enbT[b2]])
                        S.op("act", lambda e: e.copy(out=edl[p2][:, :], in_=ebT[b2][:, :, tl * 128 + 127]), reads=[B_ebT[b2]], writes=[B_edl[p2]])
                    else:
                        for h in range(4):
                            S.mm([(lambda h: lambda pe: pe.matmul(pbT[:, h:h + 1], lhsT=sp[p2][:, h * 128:(h + 1) * 128],
                                                                   rhs=triu[:, 127:128], start=True, stop=True))(h)],
                                 reads=[B_sp[p2], B_const], writes=[bbT])
                        S.op("act", lambda e: e.activation(out=edl[p2][:, :], in_=pbT[:, 0:4], func=AF.Exp), reads=[bbT], writes=[B_edl[p2]])
                    yield ("tile_pre", tl, ti, p2, (lambda p2: lambda: state_update(p2))(p2), st["u"])

            def state_update(p2):
                pd0, bd0 = ps_next()
                pd1, bd1 = ps_next()
                for h in range(4):
                    pd, bd = (pd0, bd0) if h < 2 else (pd1, bd1)
                    o = (h % 2) * 256
                    S.mm([(lambda h, pd, o: lambda pe: pe.matmul(pd[:, o:o + 256], lhsT=kdec[p2][:, h * 128:(h + 1) * 128],
                                                                  rhs=vsb[p2][:, h * 256:(h + 1) * 256], start=True, stop=True))(h, pd, o)],
                         reads=[B_kdec[p2], B_vsb[p2]], writes=[bd])
                for h in range(4):
                    pd, bd = (pd0, bd0) if h < 2 else (pd1, bd1)
                    o = (h % 2) * 256
                    S.op("dve", (lambda h, pd, o: lambda e: e.scalar_tensor_tensor(
                        out=Sst[:, h, :], in0=Sst[:, h, :], scalar=edl[p2][:, h:h + 1], in1=pd[:, o:o + 256],
                        op0=ALU.mult, op1=ALU.add))(h, pd, o), reads=[B_S, B_edl[p2], bd], writes=[B_S])
                S.op("pool", lambda e: e.tensor_copy(out=Sbf[:], in_=Sst[:]), reads=[B_S], writes=[B_Sbf])

            def main_block(blk, slot):
                b2 = blk % 2
                gen = gla_block(xT_d, blk, slot, True, True, blk == 0)
                for rc in range(8):
                    pt, pb = proj_fm(slot, RO + rc * 128, 128, 0, BLK)
                    S.op("act", (lambda rc, pt: lambda e: e.activation(out=srT[b2][:, rc, :], in_=pt[:, 0:BLK], func=AF.Silu))(rc, pt),
                         reads=[pb], writes=[B_srT[b2]])
                ev0 = next(gen)
                if pend["f"] is not None:
                    pend["f"]()
                    pend["f"] = None
                evs = [ev0] + list(gen)
                for h in range(4):
                    pq, bq = proj_fm(slot, QO + h * 128, 128, 0, BLK)
                    S.op("dve", (lambda h, pq: lambda e: e.scalar_tensor_tensor(
                        out=qtT[b2][:, h, :], in0=pq[:, 0:BLK], scalar=float(128 ** -0.5), in1=ebT[b2][:, h, :],
                        op0=ALU.mult, op1=ALU.mult))(h, pq), reads=[bq, B_ebT[b2]], writes=[B_qtT[b2]])
                    pk, bk = proj_fm(slot, KO + h * 128, 128, 0, BLK)
                    S.op("dve", (lambda h, pk: lambda e: e.tensor_tensor(
                        out=ktT[b2][:, h, :], in0=pk[:, 0:BLK], in1=enbT[b2][:, h, :], op=ALU.mult))(h, pk),
                        reads=[bk, B_enbT[b2]], writes=[B_ktT[b2]])
                for ev in evs:
                    _, tl, ti, p2, upd, ucur_ev = ev
                    tsl = slice(tl * 128, (tl + 1) * 128)
                    if pend["f"] is not None:
                        pend["f"]()
                        pend["f"] = None
                    psc, bsc = ps_next()
                    for h in range(4):
                        S.mm([(lambda h: lambda pe: pe.matmul(psc[:, h * 128:(h + 1) * 128], lhsT=ktT[b2][:, h, tsl], rhs=qtT[b2][:, h, tsl],
                                                               start=True, stop=True))(h)], reads=[B_ktT[b2], B_qtT[b2]], writes=[bsc])
                    S.op("dve", lambda e: e.tensor_tensor(out=AT[p2][:], in0=psc[:, :].rearrange("p (h i) -> p h i", h=4), in1=mask4[:],
                                                          op=ALU.mult), reads=[bsc, B_const], writes=[B_AT[p2]])
                    po = [ps_next(), ps_next()]
                    for hv in range(8):
                        h, vc = hv // 2, hv % 2
                        pt, pb = po[hv // 4]
                        o = (hv % 4) * 128
                        S.mm([(lambda h, vc, pt, o: lambda pe: pe.matmul(pt[:, o:o + 128], lhsT=vsb[p2][:, h * 256 + vc * 128:h * 256 + (vc + 1) * 128],
                                                                          rhs=AT[p2][:, h, :], start=True, stop=False))(h, vc, pt, o),
                              (lambda h, vc, pt, o: lambda pe: pe.matmul(pt[:, o:o + 128], lhsT=Sbf[:, h, vc * 128:(vc + 1) * 128],
                                                                          rhs=qtT[b2][:, h, tsl], start=False, stop=True))(h, vc, pt, o)],
                             reads=[B_vsb[p2], B_AT[p2], B_Sbf, B_qtT[b2]], writes=[pb])
                    for half in range(2):
                        pt, pb = po[half]
                        S.op("act", (lambda half, pt: lambda e: e.activation(
                            out=sq[p2][:, half * 4:(half + 1) * 4, :], in_=pt[:, :].rearrange("p (a i) -> p a i", a=4), func=AF.Square))(half, pt),
                            reads=[pb], writes=[B_sq[p2]])
                    pss, bss = ps_next()
                    for h in range(4):
                        S.mm([(lambda h, vc: lambda pe: pe.matmul(pss[:, h * 128:(h + 1) * 128], lhsT=onesb[:, :], rhs=sq[p2][:, 2 * h + vc, :],
                                                                   start=(vc == 0), stop=(vc == 1)))(h, vc) for vc in range(2)],
                             reads=[B_sq[p2], B_const], writes=[bss])
                    S.op("act", lambda e: e.activation(out=rstd[p2][:], in_=pss[:, :].rearrange("p (h i) -> p h i", h=4), func=AF.Ln,
                                                       bias=float(256 * RMS_EPS)), reads=[bss], writes=[B_rstd[p2]])
                    S.op("act", lambda e: e.activation(out=rstd[p2][:], in_=rstd[p2][:], func=AF.Exp, scale=-0.5),
                         reads=[B_rstd[p2]], writes=[B_rstd[p2]])
                    for hv in range(8):
                        h, vc = hv // 2, hv % 2
                        pt, pb = po[hv // 4]
                        o = (hv % 4) * 128
                        S.op("dve", (lambda h, vc, hv, pt, o: lambda e: e.scalar_tensor_tensor(
                            out=otmp[p2][:, hv, :], in0=pt[:, o:o + 128], scalar=g16[:, vc:vc + 1], in1=rstd[p2][:, h, :],
                            op0=ALU.mult, op1=ALU.mult))(h, vc, hv, pt, o), reads=[pb, B_rstd[p2], B_const], writes=[B_otmp[p2]])
                    S.op("pool", lambda e: e.tensor_tensor(out=ogT[b2][:, :, tsl], in0=otmp[p2][:], in1=srT[b2][:, :, tsl], op=ALU.mult),
                         reads=[B_otmp[p2], B_srT[b2]], writes=[B_ogT[b2]])
                    ucur = ucur_ev
                    uprev = (ucur + 2) % 3
                    ppl, bpl = ps_next()
                    band = bfirst if (blk == 0 and tl == 0) else bcur
                    for g in range(4):
                        S.mm([(lambda g: lambda pe: pe.matmul(ppl[:, g * 128:(g + 1) * 128], lhsT=usb[ucur][:, g * 128:(g + 1) * 128],
                                                               rhs=band[:, g, :], start=True, stop=False))(g),
                              (lambda g: lambda pe: pe.matmul(ppl[:, g * 128:(g + 1) * 128], lhsT=usb[uprev][:, g * 128:(g + 1) * 128],
                                                               rhs=bprev[:, g, :], start=False, stop=True))(g)],
                             reads=[B_usb[ucur], B_usb[uprev], B_const], writes=[bpl])
                    S.op("act", lambda e: e.copy(out=plT[p2][:], in_=ppl[:, :].rearrange("p (g t) -> p g t", g=4)), reads=[bpl], writes=[B_plT[p2]])
                    pmx, bmx = ps_next()
                    for g in range(4):
                        S.mm([(lambda g: lambda pe: pe.matmul(pmx[:, g * 128:(g + 1) * 128], lhsT=wpg[:, g, :], rhs=plT[p2][:, g, :],
                                                               start=True, stop=True))(g)], reads=[B_plT[p2], B_const2], writes=[bmx])
                    for g in range(4):
                        S.op("act", (lambda g: lambda e: e.activation(out=mxT[b2][:, g, tsl], in_=pmx[:, g * 128:(g + 1) * 128], func=AF.Copy,
                                                                       scale=pscale[:, g:g + 1]))(g), reads=[bmx, B_const], writes=[B_mxT[b2]])
                    pend["f"] = upd
                for dc in range(8):
                    d2 = dc % 2
                    pa, ba = proj_fm(slot, GAO + dc * 128, 128, 0, BLK)
                    S.op("act", (lambda d2, pa: lambda e: e.activation(out=sga[d2][:], in_=pa[:, 0:BLK], func=AF.Sigmoid))(d2, pa),
                         reads=[ba], writes=[B_sga[d2]])
                    pg, bg = proj_fm(slot, GBO + dc * 128, 128, 0, BLK)
                    S.op("act", (lambda d2, pg: lambda e: e.activation(out=sgb[d2][:], in_=pg[:, 0:BLK], func=AF.Sigmoid))(d2, pg),
                         reads=[bg], writes=[B_sgb[d2]])
                    pyg, byg = ps_next()
                    S.mm([(lambda vc, dc: lambda pe: pe.matmul(pyg[:, 0:BLK], lhsT=wglaup[:, vc, dc * 128:(dc + 1) * 128], rhs=ogT[b2][:, vc, :],
                                                                start=(vc == 0), stop=(vc == 7)))(vc, dc) for vc in range(8)],
                         reads=[B_ogT[b2], B_const2], writes=[byg])
                    pyp, byp = ps_next()
                    S.mm([(lambda pc, dc: lambda pe: pe.matmul(pyp[:, 0:BLK], lhsT=wpu[:, pc, dc * 128:(dc + 1) * 128], rhs=mxT[b2][:, pc, :],
                                                                start=(pc == 0), stop=(pc == 3)))(pc, dc) for pc in range(4)],
                         reads=[B_mxT[b2], B_const2], writes=[byp])
                    S.op("dve", (lambda dc, d2, pyg: lambda e: e.tensor_tensor(out=t1[d2][:], in0=pyg[:, 0:BLK], in1=sga[d2][:], op=ALU.mult))(dc, d2, pyg),
                         reads=[byg, B_sga[d2]], writes=[B_t1[d2]])
                    S.op("dve", (lambda dc, d2, pyp: lambda e: e.tensor_tensor(out=t2[d2][:], in0=pyp[:, 0:BLK], in1=sgb[d2][:], op=ALU.mult))(dc, d2, pyp),
                         reads=[byp, B_sgb[d2]], writes=[B_t2[d2]])
                    for tl in range(BLK // 128):
                        S.op("pool", (lambda dc, d2, tl: lambda e: e.tensor_tensor(out=mgT[b2][:, tl, dc, :], in0=t1[d2][:, tl * 128:(tl + 1) * 128],
                                                                                    in1=t2[d2][:, tl * 128:(tl + 1) * 128], op=ALU.add))(dc, d2, tl),
                             reads=[B_t1[d2], B_t2[d2]], writes=[B_mgT[b2]])
                nt2 = BLK // 128
                S.dma("sp", c_mg[b2], mT_d[:, blk * nt2:(blk + 1) * nt2, :, :], mgT[b2][:], reads=[B_mgT[b2]], writes=[B_mTd])
                if dbg:
                    S.dma("sp", c_dbg[2], dbg_d["mT"][:, blk * nt2:(blk + 1) * nt2, :, :], mgT[b2][:], reads=[B_mgT[b2]], writes=[B_mTd])

            seq = [("p", i) for i in range(NPBLK)] + [("m", i) for i in range(NBLK)]
            if stop == 'P':
                seq = seq[:NPBLK]
            if stop == 'P1':
                seq = seq[:1]

            def issue_load(idx):
                kind, i = seq[idx]
                load_x(xpT_d if kind == "p" else xT_d, i, idx % NXS)
            issue_load(0)
            for idx, (kind, i) in enumerate(seq):
                if idx + 1 < len(seq):
                    issue_load(idx + 1)
                slot = idx % NXS
                if kind == "p":
                    for ev in gla_block(xpT_d, i, slot, False, i == NPBLK - 1, False):
                        if pend["f"] is not None:
                            pend["f"]()
                        pend["f"] = ev[4]
                else:
                    main_block(i, slot)
            if pend["f"] is not None:
                pend["f"]()
                pend["f"] = None
            if dbg:
                S.dma("sp", c_dbg[3], dbg_d["S"], Sst[:], reads=[B_S])
            S.barrier()
            if stop in ('P', 'P1', 'A'):
                return nc
        with ExitStack() as esB:
            wout = sbt(esB, "wout", [128, 8, D], BF16)
            ln1g = sbt(esB, "ln1g", [128, D], F32)
            ln1b = sbt(esB, "ln1b", [128, D], F32)
            ln2g = sbt(esB, "ln2g", [128, D], F32)
            ln2b = sbt(esB, "ln2b", [128, D], F32)
            wr = sbt(esB, "wr", [128, 8, NE], F32)
            rbias = sbt(esB, "rbias", [128, NE], F32)
            c_initB = S.chan("initB")
            c_initBp = S.chan("initBp")
            B_cB = Buf("constB")
            B_cBs = Buf("constBs")
            B_cBp = Buf("constBp")
            S.dma("pool", c_initBp, wout[:], wout_d.rearrange("(c p) n -> p c n", p=128), writes=[B_cBp], last=True)
            S.dma("sp", c_initB, ln1g[:], ln1g_d, writes=[B_cBs], last=False)
            S.dma("sp", c_initB, ln1b[:], ln1b_d, writes=[B_cBs], last=False)
            S.dma("sp", c_initB, ln2g[:], ln2g_d, writes=[B_cBs], last=False)
            S.dma("sp", c_initB, ln2b[:], ln2b_d, writes=[B_cBs], last=False)
            S.dma("sp", c_initB, wr[:], wr_d.rearrange("(c p) n -> p c n", p=128), writes=[B_cBs], last=False)
            S.dma("sp", c_initB, rbias[:], rbias_d, writes=[B_cBs], last=True)

            yacc = sbt(esB, "yacc", [128, TPB, D], F32)
            B_yacc = [Buf("yacc%d" % i) for i in range(TPB)]
            hTs = [sbt(esB, "hT%d" % i, [128, 8, tb], BF16) for i in range(2)]
            B_hTs = [[Buf("hT%d_%d" % (j, i)) for i in range(NSB)] for j in range(2)]
            gatess = [sbt(esB, "gates%d" % i, [128, TPB, NE + 1], F32) for i in range(2)]
            B_gatess = [[Buf("gates%d_%d" % (j, i)) for i in range(TPB)] for j in range(2)]
            S.op("dve", lambda e: e.memset(gatess[0][:], 1.0), reads=[B_cBs, B_cBp], writes=B_gatess[0] + [B_cB])
            S.op("dve", lambda e: e.memset(gatess[1][:], 1.0), writes=B_gatess[1])
            B_h1d = [Buf("h1d%d" % i) for i in range(NT // 128)]
            c_h1 = [S.chan("h1_%d" % i) for i in range(2)]
            c_ya = [S.chan("ya%d" % i) for i in range(2)]
            NWS = 3
            wgu_e = [sbt(esB, "wgu_e%d" % i, [128, 8, 512], BF16) for i in range(NWS)]
            wd_e = [sbt(esB, "wd_e%d" % i, [128, 2, D], BF16) for i in range(NWS)]
            B_we = [Buf("we%d" % i) for i in range(NWS)]
            c_we = [S.chan("we%d" % i) for i in range(NWS)]
            mts = [sbt(esB, "mts%d" % i, [128, 8, 128], BF16) for i in range(2)]
            B_mts = [Buf("mts%d" % i) for i in range(2)]
            c_mts = [S.chan("mts%d" % i) for i in range(2)]
            xs_ = [sbt(esB, "xs%d" % i, [128, D], F32) for i in range(2)]
            B_xs = [Buf("xs%d" % i) for i in range(2)]
            c_xs = [S.chan("xs%d" % i) for i in range(2)]
            ssum = [sbt(esB, "ssum%d" % i, [128, D], F32) for i in range(2)]
            B_ssum = [Buf("ssum%d" % i) for i in range(2)]
            hf = [sbt(esB, "hf%d" % i, [128, D], F32) for i in range(2)]
            B_hf = [Buf("hf%d" % i) for i in range(2)]
            hT32 = [sbt(esB, "hT32_%d" % i, [128, 8, 128], F32) for i in range(2)]
            B_hT32 = [Buf("hT32_%d" % i) for i in range(2)]
            stats = [sbt(esB, "stats%d" % i, [128, 2, 6], F32) for i in range(2)]
            mv = [sbt(esB, "mv%d" % i, [128, 2], F32) for i in range(2)]
            rs_ = [sbt(esB, "rs%d" % i, [128, 1], F32) for i in range(2)]
            B_stats = [Buf("stats%d" % i) for i in range(2)]
            sc = [sbt(esB, "sc%d" % i, [128, NE], F32) for i in range(2)]
            bia = [sbt(esB, "bia%d" % i, [128, NE], F32) for i in range(2)]
            srt = [sbt(esB, "srt%d" % i, [128, 8, 8], F32) for i in range(2)]
            gsc = [sbt(esB, "gsc%d" % i, [128, 8], F32) for i in range(2)]
            gtop = [sbt(esB, "gtop%d" % i, [128, 8], F32) for i in range(2)]
            gm = [sbt(esB, "gm%d" % i, [128, 8], F32) for i in range(2)]
            pen = [sbt(esB, "pen%d" % i, [128, 8], F32) for i in range(2)]
            msk = [sbt(esB, "msk%d" % i, [128, NE], F32) for i in range(2)]
            etop = [sbt(esB, "etop%d" % i, [128, 8], F32) for i in range(2)]
            sel = [sbt(esB, "sel%d" % i, [128, NE], F32) for i in range(2)]
            ssc = [sbt(esB, "ssc%d" % i, [128, 1], F32) for i in range(2)]
            B_rt = [Buf("rt%d" % i) for i in range(2)]
            sgs = [sbt(esB, "sgs%d" % i, [128, 512], F32) for i in range(2)]
            B_sgs = [Buf("sgs%d" % i) for i in range(2)]
            hh = [sbt(esB, "hh%d" % i, [128, 2, 512], BF16) for i in range(2)]
            B_hh = [Buf("hh%d" % i) for i in range(2)]
            ob = [sbt(esB, "ob%d" % i, [128, D], F32) for i in range(2)]
            B_ob = [Buf("ob%d" % i) for i in range(2)]
            c_ob = [S.chan("ob%d" % i) for i in range(2)]
            B_outd = Buf("out_dram")

            def layer_norm(p2, src_ap, srcbufs, g_t, b_t, dst_ap, dstbufs, tmp_ap, tmpbuf):
                for c in range(2):
                    S.op("dve", (lambda c: lambda e: e.bn_stats(out=stats[p2][:, c, :], in_=src_ap[:, c * 512:(c + 1) * 512]))(c),
                         reads=srcbufs, writes=[B_stats[p2]])
                S.op("dve", lambda e: e.bn_aggr(out=mv[p2][:], in_=stats[p2][:].rearrange("p a b -> p (a b)")), reads=[B_stats[p2]], writes=[B_stats[p2]])
                S.op("act", lambda e: e.activation(out=rs_[p2][:], in_=mv[p2][:, 1:2], func=AF.Ln, bias=float(LN_EPS)),
                     reads=[B_stats[p2]], writes=[B_stats[p2]])
                S.op("act", lambda e: e.activation(out=rs_[p2][:], in_=rs_[p2][:], func=AF.Exp, scale=-0.5),
                     reads=[B_stats[p2]], writes=[B_stats[p2]])
                S.op("dve", lambda e: e.tensor_scalar(out=tmp_ap, in0=src_ap, scalar1=mv[p2][:, 0:1], scalar2=rs_[p2][:, 0:1],
                                                      op0=ALU.subtract, op1=ALU.mult), reads=list(srcbufs) + [B_stats[p2]], writes=[tmpbuf])
                S.op("pool", lambda e: e.tensor_tensor(out=tmp_ap, in0=tmp_ap, in1=g_t[:], op=ALU.mult), reads=[tmpbuf, B_cB], writes=[tmpbuf])
                S.op("pool", lambda e: e.tensor_tensor(out=dst_ap, in0=tmp_ap, in1=b_t[:], op=ALU.add), reads=[tmpbuf, B_cB], writes=dstbufs)

            def load_w(e, slot):
                S.dma("pool", c_we[slot], wgu_e[slot][:, :, 0:256], weg_d[e].rearrange("(c p) f -> p c f", p=128), writes=[B_we[slot]], last=False)
                S.dma("pool", c_we[slot], wgu_e[slot][:, :, 256:512], weu_d[e].rearrange("(c p) f -> p c f", p=128), writes=[B_we[slot]], last=False)
                S.dma("pool", c_we[slot], wd_e[slot][:], wed_d[e].rearrange("(f p) d -> p f d", p=128), writes=[B_we[slot]], last=True)

            wcount = {"n": 0}
            NEXP = NE + 1
            total_w = NTB * NEXP

            def issue_w(n):
                if n < total_w:
                    load_w(n % NEXP, n % NWS)

            if stop == 'B0':
                S.barrier()
                return nc
            issue_w(0)
            issue_w(1)
            gt = {"n": 0}
            ppool = {"p": (0, 8), "y": (4, 8)}

            def prologue_tile(tbi, tl, phase):
                if True:
                    hT = hTs[tbi % 2]
                    B_hT = B_hTs[tbi % 2]
                    gates = gatess[tbi % 2]
                    B_gates = B_gatess[tbi % 2]
                    gti = tbi * TPB + tl
                    p2 = gti % 2
                    sbi = tl // 4
                    if phase == 0:
                        S.dma("sp", c_mts[p2], mts[p2][:], mT_d[:, gti, :, :], writes=[B_mts[p2]])
                        S.dma("sp", c_xs[p2], xs_[p2][:], x_d[gti * 128:(gti + 1) * 128, :], writes=[B_xs[p2]])
                        return
                    if phase == 1:
                        prologue_p1(gti, p2)
                        return
                    prologue_p2(hT, B_hT, gates, B_gates, tl, p2, sbi)

            def prologue_p1(gti, p2):
                if True:
                    for half in range(2):
                        pm, bm = ps_next(*ppool["p"])
                        S.mm([(lambda dc, half: lambda pe: pe.matmul(pm[:, :], lhsT=mts[p2][:, dc, :], rhs=wout[:, dc, half * 512:(half + 1) * 512],
                                                                      start=(dc == 0), stop=(dc == 7)))(dc, half) for dc in range(8)],
                             reads=[B_mts[p2], B_cB], writes=[bm])
                        S.op("dve", (lambda half, pm: lambda e: e.scalar_tensor_tensor(
                            out=ssum[p2][:, half * 512:(half + 1) * 512], in0=xs_[p2][:, half * 512:(half + 1) * 512], scalar=ALPHA, in1=pm[:, :],
                            op0=ALU.mult, op1=ALU.add))(half, pm), reads=[B_xs[p2], bm], writes=[B_ssum[p2]])
                    layer_norm(p2, ssum[p2][:], [B_ssum[p2]], ln1g, ln1b, hf[p2][:], [B_hf[p2]], ssum[p2][:], B_ssum[p2])
                    if dbg:
                        S.dma("sp", c_dbg[p2], dbg_d["h1"][gti * 128:(gti + 1) * 128, :], hf[p2][:], reads=[B_hf[p2]])
                    S.op("act", lambda e: e.activation(out=ssum[p2][:], in_=hf[p2][:], func=AF.Copy, scale=ALPHA), reads=[B_hf[p2]], writes=[B_ssum[p2]])
                    S.dma("sp", c_h1[p2], h1_d[gti * 128:(gti + 1) * 128, :], ssum[p2][:], reads=[B_ssum[p2]], writes=[B_h1d[gti]])

            def prologue_p2(hT, B_hT, gates, B_gates, tl, p2, sbi):
                if True:
                    for half in range(2):
                        ptr, btr = ps_next(*ppool["p"])
                        for cc in range(4):
                            c = half * 4 + cc
                            S.mm([(lambda c, cc: lambda pe: pe.matmul(ptr[:, cc * 128:(cc + 1) * 128], lhsT=hf[p2][:, c * 128:(c + 1) * 128], rhs=ident[:, :], start=True, stop=True))(c, cc)],
                                 reads=[B_hf[p2], B_const], writes=[btr])
                        p3 = ptr[:, :].rearrange("p (c t) -> p c t", c=4)
                        S.op("act", (lambda half, p3: lambda e: e.copy(out=hT[:, half * 4:(half + 1) * 4, tl * 128:(tl + 1) * 128], in_=p3))(half, p3),
                             reads=[btr], writes=[B_hT[sbi]])
                        S.op("dve", (lambda half, p3: lambda e: e.tensor_copy(out=hT32[p2][:, half * 4:(half + 1) * 4, :], in_=p3))(half, p3),
                             reads=[btr], writes=[B_hT32[p2]])
                    plg, blg = ps_next(*ppool["p"])
                    S.mm([(lambda c: lambda pe: pe.matmul(plg[:, 0:NE], lhsT=hT32[p2][:, c, :], rhs=wr[:, c, :], start=(c == 0), stop=(c == 7)))(c)
                          for c in range(8)], reads=[B_hT32[p2], B_cB], writes=[blg])
                    R = [B_rt[p2]]
                    S.op("act", lambda e: e.activation(out=sc[p2][:], in_=plg[:, 0:NE], func=AF.Sigmoid), reads=[blg], writes=R)
                    S.op("dve", lambda e: e.tensor_tensor(out=bia[p2][:], in0=sc[p2][:], in1=rbias[:], op=ALU.add), reads=R + [B_cB], writes=R)
                    for g in range(8):
                        S.op("dve", (lambda g: lambda e: e.max(out=srt[p2][:, g, :], in_=bia[p2][:, g * 8:(g + 1) * 8]))(g), reads=R, writes=R)
                    S.op("dve", lambda e: e.tensor_tensor(out=gsc[p2][:], in0=srt[p2][:, :, 0], in1=srt[p2][:, :, 1], op=ALU.add), reads=R, writes=R)
                    S.op("dve", lambda e: e.max(out=gtop[p2][:], in_=gsc[p2][:]), reads=R, writes=R)
                    S.op("dve", lambda e: e.tensor_scalar(out=gm[p2][:], in0=gsc[p2][:], scalar1=gtop[p2][:, 3:4], scalar2=None, op0=ALU.is_ge), reads=R, writes=R)
                    S.op("dve", lambda e: e.tensor_scalar(out=pen[p2][:], in0=gm[p2][:], scalar1=-1.0, scalar2=1e30, op0=ALU.add, op1=ALU.mult), reads=R, writes=R)
                    for g in range(8):
                        S.op("dve", (lambda g: lambda e: e.tensor_scalar(out=msk[p2][:, g * 8:(g + 1) * 8], in0=bia[p2][:, g * 8:(g + 1) * 8],
                                                                         scalar1=gm[p2][:, g:g + 1], scalar2=pen[p2][:, g:g + 1],
                                                                         op0=ALU.mult, op1=ALU.add))(g), reads=R, writes=R)
                    S.op("dve", lambda e: e.max(out=etop[p2][:], in_=msk[p2][:]), reads=R, writes=R)
                    S.op("dve", lambda e: e.tensor_scalar(out=sel[p2][:], in0=msk[p2][:], scalar1=etop[p2][:, 7:8], scalar2=None, op0=ALU.is_ge), reads=R, writes=R)
                    S.op("dve", lambda e: e.tensor_tensor(out=sel[p2][:], in0=sel[p2][:], in1=sc[p2][:], op=ALU.mult), reads=R, writes=R)
                    S.op("dve", lambda e: e.reduce_sum(out=ssc[p2][:], in_=sel[p2][:], axis=AX.X), reads=R, writes=R)
                    S.op("dve", lambda e: e.reciprocal(out=ssc[p2][:], in_=ssc[p2][:]), reads=R, writes=R)
                    S.op("dve", lambda e: e.tensor_scalar(out=gates[:, tl, 0:NE], in0=sel[p2][:], scalar1=ssc[p2][:, 0:1], scalar2=2.5,
                                                          op0=ALU.mult, op1=ALU.mult), reads=R, writes=[B_gates[tl]])
            prologue_tile(0, 0, 0)
            for tl in range(TPB + 1):
                if tl + 1 < TPB:
                    prologue_tile(0, tl + 1, 0)
                if tl < TPB:
                    prologue_tile(0, tl, 1)
                if tl >= 1:
                    prologue_tile(0, tl - 1, 2)
            for tbi in range(NTB):
                hT = hTs[tbi % 2]
                B_hT = B_hTs[tbi % 2]
                gates = gatess[tbi % 2]
                B_gates = B_gatess[tbi % 2]
                if dbg:
                    S.dma("sp", c_dbg[3], dbg_d["gates"][:, tbi * TPB:(tbi + 1) * TPB, :], gates[:], reads=B_gates)
                if stop == 'B1':
                    S.barrier()
                    return nc
                def reload(tb2, tls):
                    for tl in tls:
                        g2 = tb2 * TPB + tl
                        S.dma("sp", c_ya[(tl // 4) % 2], yacc[:, tl, :], h1_d[g2 * 128:(g2 + 1) * 128, :], reads=[B_h1d[g2]], writes=[B_yacc[tl]],
                              last=(tl % 4 == 3 or tl == TPB - 1))

                def epilogue(tls):
                    for tl in tls:
                        gti = tbi * TPB + tl
                        p2 = gti % 2
                        layer_norm(p2, yacc[:, tl, :], [B_yacc[tl]], ln2g, ln2b, ob[p2][:], [B_ob[p2]], ob[p2][:], B_ob[p2])
                        S.dma("sp", c_ob[p2], out_d[gti * 128:(gti + 1) * 128, :], ob[p2][:], reads=[B_ob[p2]], writes=[B_outd])
                if tbi == 0:
                    reload(0, range(TPB))
                early = list(range(0, (NSB - 1) * 4))
                late = list(range((NSB - 1) * 4, TPB))
                units = [(e, sbi) for e in range(NEXP) for sbi in range(NSB)]
                pro_every = max(1, len(units) // TPB)
                if tbi + 1 < NTB:
                    ppool["p"], ppool["y"] = (4, 8), (4, 8)
                else:
                    ppool["p"], ppool["y"] = (0, 8), (4, 8)

                def emit_gu(u, fcs):
                    e, sbi = units[u]
                    n = tbi * NEXP + e
                    slot = n % NWS
                    h2 = u % 2
                    for fc in fcs:
                        pgp, bgp = ps_next(0, 4)
                        S.mm([(lambda c, fc: lambda pe: pe.matmul(pgp[:, :], lhsT=wgu_e[slot][:, c, fc * 128:(fc + 1) * 128],
                                                                   rhs=hT[:, c, sbi * 512:(sbi + 1) * 512], start=(c == 0), stop=(c == 7)))(c, fc) for c in range(8)],
                             reads=[B_we[slot], B_hT[sbi]], writes=[bgp])
                        pup, bup = ps_next(0, 4)
                        S.mm([(lambda c, fc: lambda pe: pe.matmul(pup[:, :], lhsT=wgu_e[slot][:, c, 256 + fc * 128:256 + (fc + 1) * 128],
                                                                   rhs=hT[:, c, sbi * 512:(sbi + 1) * 512], start=(c == 0), stop=(c == 7)))(c, fc) for c in range(8)],
                             reads=[B_we[slot], B_hT[sbi]], writes=[bup])
                        s2 = fc
                        S.op("act", (lambda pgp, s2: lambda e_: e_.activation(out=sgs[s2][:], in_=pgp[:, :], func=AF.Silu))(pgp, s2), reads=[bgp], writes=[B_sgs[s2]])
                        S.op("dve", (lambda pup, s2, fc: lambda e_: e_.tensor_tensor(out=hh[h2][:, fc, :], in0=pup[:, :], in1=sgs[s2][:], op=ALU.mult))(pup, s2, fc),
                             reads=[bup, B_sgs[s2]], writes=[B_hh[h2]])

                def emit_down(u, tts):
                    e, sbi = units[u]
                    n = tbi * NEXP + e
                    slot = n % NWS
                    h2 = u % 2
                    S.prewait("pe", psbuf[4:8])
                    for tt in tts:
                        tl = sbi * 4 + tt
                        for dh in range(2):
                            py, by = ps_next(*ppool["y"])
                            S.mm([(lambda fc, tt, dh: lambda pe: pe.matmul(py[:, :], lhsT=hh[h2][:, fc, tt * 128:(tt + 1) * 128],
                                                                            rhs=wd_e[slot][:, fc, dh * 512:(dh + 1) * 512], start=(fc == 0), stop=(fc == 1)))(fc, tt, dh)
                                  for fc in range(2)], reads=[B_hh[h2], B_we[slot]], writes=[by])
                            S.op("dve", (lambda py, tl, dh, e: lambda e_: e_.scalar_tensor_tensor(
                                out=yacc[:, tl, dh * 512:(dh + 1) * 512], in0=py[:, :], scalar=gates[:, tl, e:e + 1], in1=yacc[:, tl, dh * 512:(dh + 1) * 512],
                                op0=ALU.mult, op1=ALU.add))(py, tl, dh, e), reads=[by, B_gates[tl], B_yacc[tl]], writes=[B_yacc[tl]])

                for u in range(len(units)):
                    e, sbi = units[u]
                    emit_gu(u, (0,))
                    if u > 0:
                        emit_down(u - 1, (0, 1))
                    emit_gu(u, (1,))
                    if u > 0:
                        emit_down(u - 1, (2, 3))
                    if sbi == 0:
                        issue_w(tbi * NEXP + e + 2)
                    if tbi + 1 < NTB and u // pro_every < TPB:
                        ph = {1: 0, pro_every // 4 + 1: 1, (3 * pro_every) // 4: 2}.get(u % pro_every)
                        if ph is not None:
                            prologue_tile(tbi + 1, u // pro_every, ph)
                epilogue(early)
                if tbi + 1 < NTB:
                    reload(tbi + 1, early)
                emit_down(len(units) - 1, (0, 1, 2, 3))
                epilogue(late)
                if tbi + 1 < NTB:
                    reload(tbi + 1, late)
            S.wait_all_on("sp")
            S.barrier()
        print("instrs", S.ninstr, "waits", S.nwaits, flush=True)
    return nc


def _bands(first_is_seq_start):
    cur = np.zeros((128, 4, 128), np.float32)
    prev = np.zeros((128, 4, 128), np.float32)
    first = np.zeros((128, 4, 128), np.float32)
    s = np.arange(128)[:, None]
    t = np.arange(128)[None, :]
    for g, w in enumerate(POOL_W):
        inwin = ((s <= t) & (s > t - w)).astype(np.float32)
        cur[:, g, :] = inwin / w - (s == t)
        prev[:, g, :] = ((s - 128) > (t - w)).astype(np.float32) / w
        cnt = np.minimum(t + 1, w).astype(np.float32)
        first[:, g, :] = inwin / cnt - (s == t)
    if not first_is_seq_start:
        first = cur.copy()
    return cur, prev, first


def _prep_inputs(inp):
    x = np.asarray(inp["x"], np.float32)
    B, SEQ, _ = x.shape
    NSEG = 8 // B
    NT = SEQ // NSEG
    NPREV = NT * (NSEG - 1)
    f = lambda k: np.ascontiguousarray(np.asarray(inp[k], np.float32)[0])
    shared = {}
    shared["w_in"] = f("w_in")
    wgu17 = np.zeros((32, 512), np.float32)
    wgu17[0:16] = f("w_gate_up")
    wgu17[16] = f("b_gate")
    shared["wgu17"] = wgu17
    shared["gng"] = np.ascontiguousarray(f("gla_norm_g").reshape(2, 128).T)
    shared["w_gla_up"] = f("w_gla_up")
    shared["w_pool_grp"] = f("w_pool_grp")
    shared["pscale"] = np.ascontiguousarray(f("pool_scale").reshape(4, 128).T)
    shared["w_pool_up"] = f("w_pool_up")
    shared["w_out"] = f("w_out")
    for k_, n_ in (("ln1_g", "ln1g"), ("ln1_b", "ln1b"), ("ln2_g", "ln2g"), ("ln2_b", "ln2b")):
        shared[n_] = np.ascontiguousarray(np.broadcast_to(f(k_)[None, :], (128, D)))
    shared["w_router"] = f("w_router")
    shared["rbias"] = np.ascontiguousarray(np.broadcast_to(f("router_bias")[None, :], (128, NE)))
    shared["w_eg"] = np.concatenate([f("w_exp_gate"), f("w_sh_gate")[None]], 0)
    shared["w_eu"] = np.concatenate([f("w_exp_up"), f("w_sh_up")[None]], 0)
    shared["w_ed"] = np.concatenate([f("w_exp_down"), f("w_sh_down")[None]], 0)
    m = np.arange(128)[:, None]
    i = np.arange(128)[None, :]
    shared["tri_u"] = np.where(m <= i, -1.0 / 16.0, 0.0).astype(np.float32)
    shared["tri_r"] = np.where(m > i, -1.0 / 16.0, 0.0).astype(np.float32)
    mk = (m <= i).astype(np.float32)
    shared["mask4"] = np.ascontiguousarray(np.broadcast_to(mk[:, None, :], (128, 4, 128)))
    shared["ident"] = np.eye(128, dtype=np.float32)
    shared["ones"] = np.ones((128, 128), np.float32)
    in_maps = []
    for core in range(8):
        b, sgm = core // NSEG, core % NSEG
        xs = x[b, sgm * NT:(sgm + 1) * NT]
        mp = dict(shared)
        mp["x"] = np.ascontiguousarray(xs)
        mp["xT"] = np.ascontiguousarray(xs.T.reshape(8, 128, NT))
        xp = np.zeros((NPREV, D), np.float32)
        if sgm > 0:
            xp[NPREV - sgm * NT:] = x[b, 0:sgm * NT]
        mp["xpT"] = np.ascontiguousarray(xp.T.reshape(8, 128, NPREV))
        cur, prev, first = _bands(sgm == 0)
        mp["band_cur"], mp["band_prev"], mp["band_first"] = cur, prev, first
        in_maps.append(mp)
    return in_maps, B, SEQ, NT, NPREV


def kernel(**inputs):
    in_maps, B, SEQ, NT, NPREV = _prep_inputs(inputs)
    nc = build_program(NT, NPREV)
    res = run_bass_kernel_spmd(nc, in_maps, core_ids=list(range(8)))
    out = np.empty((B, SEQ, D), np.float32)
    NSEG = 8 // B
    for core in range(8):
        b, sgm = core // NSEG, core % NSEG
        out[b, sgm * NT:(sgm + 1) * NT] = res.results[core]["out"]
    return out
```

```python
import numpy as np
from contextlib import ExitStack
import concourse.bass as bass
import concourse.mybir as mybir
from concourse.bass_utils import run_bass_kernel_spmd

F32 = mybir.dt.float32
BF16 = mybir.dt.bfloat16
AF = mybir.ActivationFunctionType
ALU = mybir.AluOpType
AX = mybir.AxisListType

D = 1024
QO, KO, VO, RO, GO, UO, GAO, GBO = 0, 512, 1024, 2048, 3072, 3088, 3600, 4624
INC = 5648
NE = 64
ALPHA = float(2.0 ** 0.25)
LN_EPS = 1e-5
RMS_EPS = 1e-6
BLK = 256
TB = 1024
POOL_W = (2, 4, 8, 16)


class Buf:
    __slots__ = ("name", "w", "r", "psum")

    def __init__(self, name, psum=False):
        self.name = name
        self.w = None
        self.r = {}
        self.psum = psum


class Chan:
    def __init__(self, sem):
        self.sem = sem
        self.cnt = 0
        self.pending = []


class Sched:
    def __init__(self, nc, es):
        self.nc = nc
        self.es = es
        self.E = {"pe": nc.tensor, "act": nc.scalar, "dve": nc.vector, "pool": nc.gpsimd, "sp": nc.sync}
        self.sem = {k: es.enter_context(nc.semaphore("c_" + k)) for k in ("pe", "act", "dve", "pool")}
        self.cnt = {k: 0 for k in self.sem}
        self.waited = {k: {} for k in self.E}
        self.chans = []
        self.nwaits = 0
        self.ninstr = 0

    def chan(self, name):
        c = Chan(self.es.enter_context(self.nc.semaphore("d_" + name)))
        self.chans.append(c)
        return c

    def _need(self, e, reads, writes):
        own = self.sem.get(e)
        toks = {}

        def add(t, raw):
            if t is None:
                return
            s, v = t
            if s is own and (e == "pe" or not raw):
                return
            if toks.get(s, 0) < v:
                toks[s] = v
        for b in reads:
            add(b.w, True)
            if b.psum:
                for s, v in b.r.items():
                    add((s, v), False)
        for b in writes:
            add(b.w, False)
            for s, v in b.r.items():
                add((s, v), False)
        wd = self.waited[e]
        for s, v in toks.items():
            if wd.get(s, 0) >= v:
                continue
            self.E[e].wait_ge(s, v)
            wd[s] = v
            self.nwaits += 1

    def prewait(self, e, writes):
        self._need(e, (), writes)

    def _done(self, tok, reads, writes):
        for b in reads:
            b.r[tok[0]] = tok[1]
        for b in writes:
            b.w = tok
            b.r = {}

    def op(self, e, fn, reads=(), writes=()):
        self._need(e, reads, writes)
        ins = fn(self.E[e])
        self.cnt[e] += 1
        ins.then_inc(self.sem[e], 1)
        self.ninstr += 1
        self._done((self.sem[e], self.cnt[e]), reads, writes)

    def mm(self, fns, reads, writes):
        self._need("pe", reads, writes)
        ins = None
        for fn in fns:
            ins = fn(self.nc.tensor)
            self.ninstr += 1
        self.cnt["pe"] += 1
        ins.then_inc(self.sem["pe"], 1)
        self._done((self.sem["pe"], self.cnt["pe"]), reads, writes)

    def dma(self, q, chan, out, in_, reads=(), writes=(), last=True):
        self._need(q, reads, writes)
        ins = self.E[q].dma_start(out=out, in_=in_)
        chan.cnt += 16
        ins.then_inc(chan.sem, 16)
        self.ninstr += 1
        chan.pending.append((tuple(reads), tuple(writes)))
        if last:
            tok = (chan.sem, chan.cnt)
            for r, w in chan.pending:
                self._done(tok, r, w)
            chan.pending = []

    def barrier(self):
        for e in self.E:
            wd = self.waited[e]
            for k, s in self.sem.items():
                if k == e:
                    continue
                if wd.get(s, 0) < self.cnt[k]:
                    self.E[e].wait_ge(s, self.cnt[k])
                    wd[s] = self.cnt[k]
            for c in self.chans:
                if c.cnt and wd.get(c.sem, 0) < c.cnt:
                    self.E[e].wait_ge(c.sem, c.cnt)
                    wd[c.sem] = c.cnt

    def wait_all_on(self, e):
        wd = self.waited[e]
        for k, s in self.sem.items():
            if wd.get(s, 0) < self.cnt[k]:
                self.E[e].wait_ge(s, self.cnt[k])
                wd[s] = self.cnt[k]
        for c in self.chans:
            if c.cnt and wd.get(c.sem, 0) < c.cnt:
                self.E[e].wait_ge(c.sem, c.cnt)
                wd[c.sem] = c.cnt


def build_program(NT, NPREV, dbg=False, stop=None):
    nc = bass.Bass("TRN2", target_bir_lowering=False)
    NBLK = NT // BLK
    NPBLK = NPREV // BLK
    tb = min(TB, NT)
    NTB = NT // tb
    TPB = tb // 128
    NSB = tb // 512

    def din(name, shape):
        return nc.dram_tensor(name, list(shape), F32, kind="ExternalInput").ap()

    xT_d = din("xT", [8, 128, NT])
    x_d = din("x", [NT, D])
    xpT_d = din("xpT", [8, 128, NPREV])
    w_in_d = din("w_in", [D, INC])
    wgu_d = din("wgu17", [32, 512])
    gng_d = din("gng", [128, 2])
    wglaup_d = din("w_gla_up", [D, D])
    wpg_d = din("w_pool_grp", [4, 128, 128])
    pscale_d = din("pscale", [128, 4])
    wpu_d = din("w_pool_up", [512, D])
    wout_d = din("w_out", [D, D])
    ln1g_d = din("ln1g", [128, D])
    ln1b_d = din("ln1b", [128, D])
    ln2g_d = din("ln2g", [128, D])
    ln2b_d = din("ln2b", [128, D])
    wr_d = din("w_router", [D, NE])
    rbias_d = din("rbias", [128, NE])
    weg_d = din("w_eg", [NE + 1, D, 256])
    weu_d = din("w_eu", [NE + 1, D, 256])
    wed_d = din("w_ed", [NE + 1, 256, D])
    triu_d = din("tri_u", [128, 128])
    trir_d = din("tri_r", [128, 128])
    mask4_d = din("mask4", [128, 4, 128])
    ident_d = din("ident", [128, 128])
    ones_d = din("ones", [128, 128])
    bcur_d = din("band_cur", [128, 4, 128])
    bprev_d = din("band_prev", [128, 4, 128])
    bfirst_d = din("band_first", [128, 4, 128])
    out_d = nc.dram_tensor("out", [NT, D], F32, kind="ExternalOutput").ap()
    mT_d = nc.dram_tensor("mT_scr", [128, NT // 128, 8, 128], BF16, kind="Internal").ap()
    h1_d = nc.dram_tensor("h1_scr", [NT, D], F32, kind="Internal").ap()
    dbg_d = {}
    if dbg:
        dbg_d["h1"] = nc.dram_tensor("dbg_h1", [NT, D], F32, kind="ExternalOutput").ap()
        dbg_d["mT"] = nc.dram_tensor("dbg_mT", [128, NT // 128, 8, 128], BF16, kind="ExternalOutput").ap()
        dbg_d["S"] = nc.dram_tensor("dbg_S", [128, 4, 256], F32, kind="ExternalOutput").ap()
        dbg_d["gates"] = nc.dram_tensor("dbg_gates", [128, NT // 128, NE + 1], F32, kind="ExternalOutput").ap()

    with ExitStack() as es0:
        S = Sched(nc, es0)
        psb = [es0.enter_context(nc.psum_tensor("ps%d" % i, [128, 512], F32)) for i in range(8)]
        psbuf = [Buf("ps%d" % i, psum=True) for i in range(8)]
        rr = {"i": 0}

        def ps_next(lo=0, hi=8):
            i = rr.setdefault((lo, hi), lo)
            rr[(lo, hi)] = lo + ((i - lo + 1) % (hi - lo))
            return psb[i], psbuf[i]

        def sbt(es, name, shape, dt):
            return es.enter_context(nc.sbuf_tensor("s_" + name, list(shape), dt))

        triu = sbt(es0, "triu", [128, 128], F32)
        trir = sbt(es0, "trir", [128, 128], F32)
        ident = sbt(es0, "ident", [128, 128], F32)
        onesb = sbt(es0, "onesb", [128, 128], BF16)
        Sst = sbt(es0, "Sst", [128, 4, 256], F32)
        Sbf = sbt(es0, "Sbf", [128, 4, 256], BF16)
        c_init = S.chan("init")
        c_initp = S.chan("initp")
        c_dbg = [S.chan("dbg%d" % i) for i in range(4)] if dbg else None
        B_const = Buf("const")
        B_csp = Buf("const_sp")
        B_cpool = Buf("const_pool")
        B_S = Buf("S")
        B_Sbf = Buf("Sbf")
        S.dma("sp", c_init, triu[:], triu_d, writes=[B_csp], last=False)
        S.dma("sp", c_init, trir[:], trir_d, writes=[B_csp], last=False)
        S.dma("sp", c_init, ident[:], ident_d, writes=[B_csp], last=False)
        S.dma("pool", c_initp, onesb[:], ones_d, writes=[B_cpool], last=False)
        S.op("dve", lambda e: e.memset(Sst[:], 0.0), writes=[B_S])
        S.op("dve", lambda e: e.memset(Sbf[:], 0.0), writes=[B_Sbf])

        with ExitStack() as esA:
            w_in = sbt(esA, "w_in", [128, 8, INC], BF16)
            wgu = sbt(esA, "wgu", [32, 512], BF16)
            wglaup = sbt(esA, "wglaup", [128, 8, D], BF16)
            wpg = sbt(esA, "wpg", [128, 4, 128], BF16)
            wpu = sbt(esA, "wpu", [128, 4, D], BF16)
            gng = sbt(esA, "gng", [128, 2], F32)
            g16 = sbt(esA, "g16", [128, 2], F32)
            pscale = sbt(esA, "pscale", [128, 4], F32)
            mask4 = sbt(esA, "mask4", [128, 4, 128], BF16)
            bcur = sbt(esA, "bcur", [128, 4, 128], BF16)
            bprev = sbt(esA, "bprev", [128, 4, 128], BF16)
            bfirst = sbt(esA, "bfirst", [128, 4, 128], BF16)
            w_in_v = w_in_d.rearrange("(c p) n -> p c n", p=128)
            S.dma("pool", c_initp, wgu[:], wgu_d, writes=[B_cpool], last=False)
            for (lo, hi) in ((GO, GO + 16), (KO, KO + 512), (VO, VO + 1024), (UO, UO + 512)):
                S.dma("pool", c_initp, w_in[:, :, lo:hi], w_in_v[:, :, lo:hi], writes=[B_cpool], last=False)
            S.dma("sp", c_init, gng[:], gng_d, writes=[B_csp], last=False)
            S.dma("sp", c_init, pscale[:], pscale_d, writes=[B_csp], last=True)
            S.dma("pool", c_initp, mask4[:], mask4_d, writes=[B_cpool], last=False)
            S.dma("pool", c_initp, bcur[:], bcur_d, writes=[B_cpool], last=False)
            S.dma("pool", c_initp, bprev[:], bprev_d, writes=[B_cpool], last=False)
            S.dma("pool", c_initp, bfirst[:], bfirst_d, writes=[B_cpool], last=True)
            c_init2 = S.chan("init2")
            B_const2 = Buf("const2")
            for (lo, hi) in ((QO, QO + 512), (RO, RO + 1024), (GAO, GAO + 1024), (GBO, GBO + 1024)):
                S.dma("pool", c_init2, w_in[:, :, lo:hi], w_in_v[:, :, lo:hi], writes=[B_const2], last=False)
            S.dma("pool", c_init2, wglaup[:], wglaup_d.rearrange("(c p) n -> p c n", p=128), writes=[B_const2], last=False)
            S.dma("pool", c_init2, wpg[:], wpg_d.rearrange("g c e -> c g e"), writes=[B_const2], last=False)
            S.dma("pool", c_init2, wpu[:], wpu_d.rearrange("(c p) n -> p c n", p=128), writes=[B_const2], last=True)
            S.op("dve", lambda e: e.tensor_scalar_mul(out=g16[:], in0=gng[:], scalar1=16.0), reads=[B_csp, B_cpool], writes=[B_const])

            if stop == 'init':
                S.barrier()
                return nc
            NXS = 2
            xTb = [sbt(esA, "xTb%d" % i, [128, 8, BLK], BF16) for i in range(NXS)]
            B_xTb = [Buf("xTb%d" % i) for i in range(NXS)]
            c_x = [S.chan("x%d" % i) for i in range(NXS)]
            glr = [sbt(esA, "glr%d" % i, [32, BLK], BF16) for i in range(2)]
            B_glr = [Buf("glr%d" % i) for i in range(2)]
            for i in range(2):
                S.op("dve", (lambda i: lambda e: e.memset(glr[i][:], 1.0))(i), writes=[B_glr[i]])
            e1 = sbt(esA, "e1", [128, 512], F32)
            B_e1 = Buf("e1")
            sp = [sbt(esA, "sp%d" % i, [128, 512], F32) for i in range(1)] * 2
            B_sp = [Buf("sp%d" % i) for i in range(1)] * 2
            ebT = [sbt(esA, "ebT%d" % i, [128, 4, BLK], F32) for i in range(1)] * 2
            B_ebT = [Buf("ebT%d" % i) for i in range(1)] * 2
            enbT = [sbt(esA, "enbT%d" % i, [128, 4, BLK], F32) for i in range(1)] * 2
            B_enbT = [Buf("enbT%d" % i) for i in range(1)] * 2
            edl = [sbt(esA, "edl%d" % i, [128, 4], F32) for i in range(2)]
            B_edl = [Buf("edl%d" % i) for i in range(2)]
            erb = [sbt(esA, "erb%d" % i, [128, 512], F32) for i in range(1)] * 2
            B_erb = [Buf("erb%d" % i) for i in range(1)] * 2
            kdec = [sbt(esA, "kdec%d" % i, [128, 512], BF16) for i in range(2)]
            B_kdec = [Buf("kdec%d" % i) for i in range(2)]
            vsb = [sbt(esA, "vsb%d" % i, [128, 1024], BF16) for i in range(2)]
            B_vsb = [Buf("vsb%d" % i) for i in range(2)]
            usb = [sbt(esA, "usb%d" % i, [128, 512], BF16) for i in range(3)]
            B_usb = [Buf("usb%d" % i) for i in range(3)]
            qtT = [sbt(esA, "qtT%d" % i, [128, 4, BLK], BF16) for i in range(1)] * 2
            B_qtT = [Buf("qtT%d" % i) for i in range(1)] * 2
            ktT = [sbt(esA, "ktT%d" % i, [128, 4, BLK], BF16) for i in range(1)] * 2
            B_ktT = [Buf("ktT%d" % i) for i in range(1)] * 2
            srT = [sbt(esA, "srT%d" % i, [128, 8, BLK], BF16) for i in range(1)] * 2
            B_srT = [Buf("srT%d" % i) for i in range(1)] * 2
            sga = [sbt(esA, "sga%d" % i, [128, BLK], BF16) for i in range(2)]
            B_sga = [Buf("sga%d" % i) for i in range(2)]
            sgb = [sbt(esA, "sgb%d" % i, [128, BLK], BF16) for i in range(2)]
            B_sgb = [Buf("sgb%d" % i) for i in range(2)]
            AT = [sbt(esA, "AT%d" % i, [128, 4, 128], BF16) for i in range(2)]
            B_AT = [Buf("AT%d" % i) for i in range(2)]
            sq = [sbt(esA, "sq%d" % i, [128, 8, 128], BF16) for i in range(1)] * 2
            B_sq = [Buf("sq%d" % i) for i in range(1)] * 2
            rstd = [sbt(esA, "rstd%d" % i, [128, 4, 128], F32) for i in range(1)] * 2
            B_rstd = [Buf("rstd%d" % i) for i in range(1)] * 2
            otmp = [sbt(esA, "otmp%d" % i, [128, 8, 128], F32) for i in range(1)] * 2
            B_otmp = [Buf("otmp%d" % i) for i in range(1)] * 2
            ogT = [sbt(esA, "ogT%d" % i, [128, 8, BLK], BF16) for i in range(1)] * 2
            B_ogT = [Buf("ogT%d" % i) for i in range(1)] * 2
            plT = [sbt(esA, "plT%d" % i, [128, 4, 128], BF16) for i in range(2)]
            B_plT = [Buf("plT%d" % i) for i in range(2)]
            mxT = [sbt(esA, "mxT%d" % i, [128, 4, BLK], BF16) for i in range(2)]
            B_mxT = [Buf("mxT%d" % i) for i in range(2)]
            t1 = [sbt(esA, "t1_%d" % i, [128, BLK], F32) for i in range(1)] * 2
            B_t1 = [Buf("t1_%d" % i) for i in range(1)] * 2
            t2 = [sbt(esA, "t2_%d" % i, [128, BLK], F32) for i in range(1)] * 2
            B_t2 = [Buf("t2_%d" % i) for i in range(1)] * 2
            mgT = [sbt(esA, "mgT%d" % i, [128, 2, 8, 128], BF16) for i in range(1)] * 2
            B_mgT = [Buf("mgT%d" % i) for i in range(1)] * 2
            c_mg = [S.chan("mg0")] * 2
            B_mTd = Buf("mT_dram")

            st = {"tile": 0, "u": 0}
            pend = {"f": None}

            def load_x(src, blk, slot):
                S.dma("pool", c_x[slot], xTb[slot][:], src[:, :, blk * BLK:(blk + 1) * BLK].rearrange("c p t -> p c t"),
                      writes=[B_xTb[slot]])

            def proj_fm(slot, col0, M, n0, n1):
                pt, pb = ps_next()
                xs = xTb[slot]
                S.mm([(lambda c: lambda pe: pe.matmul(pt[0:M, 0:n1 - n0], lhsT=w_in[:, c, col0:col0 + M],
                                                       rhs=xs[:, c, n0:n1], start=(c == 0), stop=(c == 7)))(c)
                      for c in range(8)], reads=[B_xTb[slot], B_const, B_const2], writes=[pb])
                return pt, pb

            def proj_tm(slot, col0, N, tl):
                pt, pb = ps_next()
                xs = xTb[slot]
                S.mm([(lambda c: lambda pe: pe.matmul(pt[:, 0:N], lhsT=xs[:, c, tl * 128:(tl + 1) * 128],
                                                       rhs=w_in[:, c, col0:col0 + N], start=(c == 0), stop=(c == 7)))(c)
                      for c in range(8)], reads=[B_xTb[slot], B_const, B_const2], writes=[pb])
                return pt, pb

            def gla_block(src, blk, slot, main, want_u, first_main_blk):
                par = blk % 2 if main else (blk % 2)
                b2 = blk % 2
                pt, pb = proj_fm(slot, GO, 16, 0, BLK)
                S.op("act", lambda e: e.copy(out=glr[b2][0:16, :], in_=pt[0:16, 0:BLK]), reads=[pb], writes=[B_glr[b2]])
                for tl in range(BLK // 128):
                    ti = st["tile"]
                    st["tile"] += 1
                    p2 = ti % 2
                    tsl = slice(tl * 128, (tl + 1) * 128)
                    pz, bz = ps_next()
                    S.mm([lambda pe: pe.matmul(pz[:, :], lhsT=glr[b2][0:17, tsl], rhs=wgu[0:17, :], start=True, stop=True)],
                         reads=[B_glr[b2], B_const], writes=[bz])
                    S.op("act", lambda e: e.activation(out=e1[:], in_=pz[:, :], func=AF.Exp, scale=-1.0), reads=[bz], writes=[B_e1])
                    S.op("act", lambda e: e.activation(out=sp[p2][:], in_=e1[:], func=AF.Ln, bias=1.0), reads=[B_e1], writes=[B_sp[p2]])
                    pk, bk = proj_tm(slot, KO, 512, tl)
                    pv0, bv0 = proj_tm(slot, VO, 512, tl)
                    pv1, bv1 = proj_tm(slot, VO + 512, 512, tl)
                    S.op("act", lambda e: e.copy(out=vsb[p2][:, 0:512], in_=pv0[:, :]), reads=[bv0], writes=[B_vsb[p2]])
                    S.op("dve", lambda e: e.tensor_copy(out=vsb[p2][:, 512:1024], in_=pv1[:, :]), reads=[bv1], writes=[B_vsb[p2]])
                    if want_u:
                        st["u"] = (st["u"] + 1) % 3
                        ui = st["u"]
                        pu, bu = proj_tm(slot, UO, 512, tl)
                        S.op("act", lambda e: e.copy(out=usb[ui][:], in_=pu[:, :]), reads=[bu], writes=[B_usb[ui]])
                    prb, brb = ps_next()
                    S.mm([lambda pe: pe.matmul(prb[:, :], lhsT=trir[:, :], rhs=sp[p2][:, :], start=True, stop=True)],
                         reads=[B_sp[p2], B_const], writes=[brb])
                    S.op("act", lambda e: e.activation(out=erb[p2][:], in_=prb[:, :], func=AF.Exp), reads=[brb], writes=[B_erb[p2]])
                    S.op("dve", lambda e: e.tensor_tensor(out=kdec[p2][:], in0=pk[:, :], in1=erb[p2][:], op=ALU.mult),
                         reads=[bk, B_erb[p2]], writes=[B_kdec[p2]])
                    pbT, bbT = ps_next()
                    if main:
                        for h in range(4):
                            S.mm([(lambda h: lambda pe: pe.matmul(pbT[:, h * 128:(h + 1) * 128], lhsT=sp[p2][:, h * 128:(h + 1) * 128],
                                                                   rhs=triu[:, :], start=True, stop=True))(h)],
                                 reads=[B_sp[p2], B_const], writes=[bbT])
                        pb3 = pbT[:, :].rearrange("p (h i) -> p h i", h=4)
                        S.op("act", lambda e: e.activation(out=ebT[b2][:, :, tsl], in_=pb3, func=AF.Exp), reads=[bbT], writes=[B_ebT[b2]])
                        S.op("act", lambda e: e.activation(out=enbT[b2][:, :, tsl], in_=pb3, func=AF.Exp, scale=-1.0), reads=[bbT], writes=[B_enbT[b2]])
                        S.op("act", lambda e: e.copy(out=edl[p2][:, :], in_=ebT[b2][:, :, tl * 128 + 127]), reads=[B_ebT[b2]], writes=[B_edl[p2]])
                    else:
                        for h in range(4):
                            S.mm([(lambda h: lambda pe: pe.matmul(pbT[:, h:h + 1], lhsT=sp[p2][:, h * 128:(h + 1) * 128],
                                                                   rhs=triu[:, 127:128], start=True, stop=True))(h)],
                                 reads=[B_sp[p2], B_const], writes=[bbT])
                        S.op("act", lambda e: e.activation(out=edl[p2][:, :], in_=pbT[:, 0:4], func=AF.Exp), reads=[bbT], writes=[B_edl[p2]])
                    yield ("tile_pre", tl, ti, p2, (lambda p2: lambda: state_update(p2))(p2), st["u"])

            def state_update(p2):
                pd0, bd0 = ps_next()
                pd1, bd1 = ps_next()
                for h in range(4):
                    pd, bd = (pd0, bd0) if h < 2 else (pd1, bd1)
                    o = (h % 2) * 256
                    S.mm([(lambda h, pd, o: lambda pe: pe.matmul(pd[:, o:o + 256], lhsT=kdec[p2][:, h * 128:(h + 1) * 128],
                                                                  rhs=vsb[p2][:, h * 256:(h + 1) * 256], start=True, stop=True))(h, pd, o)],
                         reads=[B_kdec[p2], B_vsb[p2]], writes=[bd])
                for h in range(4):
                    pd, bd = (pd0, bd0) if h < 2 else (pd1, bd1)
                    o = (h % 2) * 256
                    S.op("dve", (lambda h, pd, o: lambda e: e.scalar_tensor_tensor(
                        out=Sst[:, h, :], in0=Sst[:, h, :], scalar=edl[p2][:, h:h + 1], in1=pd[:, o:o + 256],
                        op0=ALU.mult, op1=ALU.add))(h, pd, o), reads=[B_S, B_edl[p2], bd], writes=[B_S])
                S.op("pool", lambda e: e.tensor_copy(out=Sbf[:], in_=Sst[:]), reads=[B_S], writes=[B_Sbf])

            def main_block(blk, slot):
                b2 = blk % 2
                gen = gla_block(xT_d, blk, slot, True, True, blk == 0)
                for rc in range(8):
                    pt, pb = proj_fm(slot, RO + rc * 128, 128, 0, BLK)
                    S.op("act", (lambda rc, pt: lambda e: e.activation(out=srT[b2][:, rc, :], in_=pt[:, 0:BLK], func=AF.Silu))(rc, pt),
                         reads=[pb], writes=[B_srT[b2]])
                ev0 = next(gen)
                if pend["f"] is not None:
                    pend["f"]()
                    pend["f"] = None
                evs = [ev0] + list(gen)
                for h in range(4):
                    pq, bq = proj_fm(slot, QO + h * 128, 128, 0, BLK)
                    S.op("dve", (lambda h, pq: lambda e: e.scalar_tensor_tensor(
                        out=qtT[b2][:, h, :], in0=pq[:, 0:BLK], scalar=float(128 ** -0.5), in1=ebT[b2][:, h, :],
                        op0=ALU.mult, op1=ALU.mult))(h, pq), reads=[bq, B_ebT[b2]], writes=[B_qtT[b2]])
                    pk, bk = proj_fm(slot, KO + h * 128, 128, 0, BLK)
                    S.op("dve", (lambda h, pk: lambda e: e.tensor_tensor(
                        out=ktT[b2][:, h, :], in0=pk[:, 0:BLK], in1=enbT[b2][:, h, :], op=ALU.mult))(h, pk),
                        reads=[bk, B_enbT[b2]], writes=[B_ktT[b2]])
                for ev in evs:
                    _, tl, ti, p2, upd, ucur_ev = ev
                    tsl = slice(tl * 128, (tl + 1) * 128)
                    if pend["f"] is not None:
                        pend["f"]()
                        pend["f"] = None
                    psc, bsc = ps_next()
                    for h in range(4):
                        S.mm([(lambda h: lambda pe: pe.matmul(psc[:, h * 128:(h + 1) * 128], lhsT=ktT[b2][:, h, tsl], rhs=qtT[b2][:, h, tsl],
                                                               start=True, stop=True))(h)], reads=[B_ktT[b2], B_qtT[b2]], writes=[bsc])
                    S.op("dve", lambda e: e.tensor_tensor(out=AT[p2][:], in0=psc[:, :].rearrange("p (h i) -> p h i", h=4), in1=mask4[:],
                                                          op=ALU.mult), reads=[bsc, B_const], writes=[B_AT[p2]])
                    po = [ps_next(), ps_next()]
                    for hv in range(8):
                        h, vc = hv // 2, hv % 2
                        pt, pb = po[hv // 4]
                        o = (hv % 4) * 128
                        S.mm([(lambda h, vc, pt, o: lambda pe: pe.matmul(pt[:, o:o + 128], lhsT=vsb[p2][:, h * 256 + vc * 128:h * 256 + (vc + 1) * 128],
                                                                          rhs=AT[p2][:, h, :], start=True, stop=False))(h, vc, pt, o),
                              (lambda h, vc, pt, o: lambda pe: pe.matmul(pt[:, o:o + 128], lhsT=Sbf[:, h, vc * 128:(vc + 1) * 128],
                                                                          rhs=qtT[b2][:, h, tsl], start=False, stop=True))(h, vc, pt, o)],
                             reads=[B_vsb[p2], B_AT[p2], B_Sbf, B_qtT[b2]], writes=[pb])
                    for half in range(2):
                        pt, pb = po[half]
                        S.op("act", (lambda half, pt: lambda e: e.activation(
                            out=sq[p2][:, half * 4:(half + 1) * 4, :], in_=pt[:, :].rearrange("p (a i) -> p a i", a=4), func=AF.Square))(half, pt),
                            reads=[pb], writes=[B_sq[p2]])
                    pss, bss = ps_next()
                    for h in range(4):
                        S.mm([(lambda h, vc: lambda pe: pe.matmul(pss[:, h * 128:(h + 1) * 128], lhsT=onesb[:, :], rhs=sq[p2][:, 2 * h + vc, :],
                                                                   start=(vc == 0), stop=(vc == 1)))(h, vc) for vc in range(2)],
                             reads=[B_sq[p2], B_const], writes=[bss])
                    S.op("act", lambda e: e.activation(out=rstd[p2][:], in_=pss[:, :].rearrange("p (h i) -> p h i", h=4), func=AF.Ln,
                                                       bias=float(256 * RMS_EPS)), reads=[bss], writes=[B_rstd[p2]])
                    S.op("act", lambda e: e.activation(out=rstd[p2][:], in_=rstd[p2][:], func=AF.Exp, scale=-0.5),
                         reads=[B_rstd[p2]], writes=[B_rstd[p2]])
                    for hv in range(8):
                        h, vc = hv // 2, hv % 2
                        pt, pb = po[hv // 4]
                        o = (hv % 4) * 128
                        S.op("dve", (lambda h, vc, hv, pt, o: lambda e: e.scalar_tensor_tensor(
                            out=otmp[p2][:, hv, :], in0=pt[:, o:o + 128], scalar=g16[:, vc:vc + 1], in1=rstd[p2][:, h, :],
                            op0=ALU.mult, op1=ALU.mult))(h, vc, hv, pt, o), reads=[pb, B_rstd[p2], B_const], writes=[B_otmp[p2]])
                    S.op("pool", lambda e: e.tensor_tensor(out=ogT[b2][:, :, tsl], in0=otmp[p2][:], in1=srT[b2][:, :, tsl], op=ALU.mult),
                         reads=[B_otmp[p2], B_srT[b2]], writes=[B_ogT[b2]])
                    ucur = ucur_ev
                    uprev = (ucur + 2) % 3
                    ppl, bpl = ps_next()
                    band = bfirst if (blk == 0 and tl == 0) else bcur
                    for g in range(4):
                        S.mm([(lambda g: lambda pe: pe.matmul(ppl[:, g * 128:(g + 1) * 128], lhsT=usb[ucur][:, g * 128:(g + 1) * 128],
                                                               rhs=band[:, g, :], start=True, stop=False))(g),
                              (lambda g: lambda pe: pe.matmul(ppl[:, g * 128:(g + 1) * 128], lhsT=usb[uprev][:, g * 128:(g + 1) * 128],
                                                               rhs=bprev[:, g, :], start=False, stop=True))(g)],
                             reads=[B_usb[ucur], B_usb[uprev], B_const], writes=[bpl])
                    S.op("act", lambda e: e.copy(out=plT[p2][:], in_=ppl[:, :].rearrange("p (g t) -> p g t", g=4)), reads=[bpl], writes=[B_plT[p2]])
                    pmx, bmx = ps_next()
                    for g in range(4):
                        S.mm([(lambda g: lambda pe: pe.matmul(pmx[:, g * 128:(g + 1) * 128], lhsT=wpg[:, g, :], rhs=plT[p2][:, g, :],
                                                               start=True, stop=True))(g)], reads=[B_plT[p2], B_const2], writes=[bmx])
                    for g in range(4):
                        S.op("act", (lambda g: lambda e: e.activation(out=mxT[b2][:, g, tsl], in_=pmx[:, g * 128:(g + 1) * 128], func=AF.Copy,
                                                                       scale=pscale[:, g:g + 1]))(g), reads=[bmx, B_const], writes=[B_mxT[b2]])
                    pend["f"] = upd
                for dc in range(8):
                    d2 = dc % 2
                    pa, ba = proj_fm(slot, GAO + dc * 128, 128, 0, BLK)
                    S.op("act", (lambda d2, pa: lambda e: e.activation(out=sga[d2][:], in_=pa[:, 0:BLK], func=AF.Sigmoid))(d2, pa),
                         reads=[ba], writes=[B_sga[d2]])
                    pg, bg = proj_fm(slot, GBO + dc * 128, 128, 0, BLK)
                    S.op("act", (lambda d2, pg: lambda e: e.activation(out=sgb[d2][:], in_=pg[:, 0:BLK], func=AF.Sigmoid))(d2, pg),
                         reads=[bg], writes=[B_sgb[d2]])
                    pyg, byg = ps_next()
                    S.mm([(lambda vc, dc: lambda pe: pe.matmul(pyg[:, 0:BLK], lhsT=wglaup[:, vc, dc * 128:(dc + 1) * 128], rhs=ogT[b2][:, vc, :],
                                                                start=(vc == 0), stop=(vc == 7)))(vc, dc) for vc in range(8)],
                         reads=[B_ogT[b2], B_const2], writes=[byg])
                    pyp, byp = ps_next()
                    S.mm([(lambda pc, dc: lambda pe: pe.matmul(pyp[:, 0:BLK], lhsT=wpu[:, pc, dc * 128:(dc + 1) * 128], rhs=mxT[b2][:, pc, :],
                                                                start=(pc == 0), stop=(pc == 3)))(pc, dc) for pc in range(4)],
                         reads=[B_mxT[b2], B_const2], writes=[byp])
                    S.op("dve", (lambda dc, d2, pyg: lambda e: e.tensor_tensor(out=t1[d2][:], in0=pyg[:, 0:BLK], in1=sga[d2][:], op=ALU.mult))(dc, d2, pyg),
                         reads=[byg, B_sga[d2]], writes=[B_t1[d2]])
                    S.op("dve", (lambda dc, d2, pyp: lambda e: e.tensor_tensor(out=t2[d2][:], in0=pyp[:, 0:BLK], in1=sgb[d2][:], op=ALU.mult))(dc, d2, pyp),
                         reads=[byp, B_sgb[d2]], writes=[B_t2[d2]])
                    for tl in range(BLK // 128):
                        S.op("pool", (lambda dc, d2, tl: lambda e: e.tensor_tensor(out=mgT[b2][:, tl, dc, :], in0=t1[d2][:, tl * 128:(tl + 1) * 128],
                                                                                    in1=t2[d2][:, tl * 128:(tl + 1) * 128], op=ALU.add))(dc, d2, tl),
                             reads=[B_t1[d2], B_t2[d2]], writes=[B_mgT[b2]])
                nt2 = BLK // 128
                S.dma("sp", c_mg[b2], mT_d[:, blk * nt2:(blk + 1) * nt2, :, :], mgT[b2][:], reads=[B_mgT[b2]], writes=[B_mTd])
                if dbg:
                    S.dma("sp", c_dbg[2], dbg_d["mT"][:, blk * nt2:(blk + 1) * nt2, :, :], mgT[b2][:], reads=[B_mgT[b2]], writes=[B_mTd])

            seq = [("p", i) for i in range(NPBLK)] + [("m", i) for i in range(NBLK)]
            if stop == 'P':
                seq = seq[:NPBLK]
            if stop == 'P1':
                seq = seq[:1]

            def issue_load(idx):
                kind, i = seq[idx]
                load_x(xpT_d if kind == "p" else xT_d, i, idx % NXS)
            issue_load(0)
            for idx, (kind, i) in enumerate(seq):
                if idx + 1 < len(seq):
                    issue_load(idx + 1)
                slot = idx % NXS
                if kind == "p":
                    for ev in gla_block(xpT_d, i, slot, False, i == NPBLK - 1, False):
                        if pend["f"] is not None:
                            pend["f"]()
                        pend["f"] = ev[4]
                else:
                    main_block(i, slot)
            if pend["f"] is not None:
                pend["f"]()
                pend["f"] = None
            if dbg:
                S.dma("sp", c_dbg[3], dbg_d["S"], Sst[:], reads=[B_S])
            S.barrier()
            if stop in ('P', 'P1', 'A'):
                return nc
        with ExitStack() as esB:
            wout = sbt(esB, "wout", [128, 8, D], BF16)
            ln1g = sbt(esB, "ln1g", [128, D], F32)
            ln1b = sbt(esB, "ln1b", [128, D], F32)
            ln2g = sbt(esB, "ln2g", [128, D], F32)
            ln2b = sbt(esB, "ln2b", [128, D], F32)
            wr = sbt(esB, "wr", [128, 8, NE], F32)
            rbias = sbt(esB, "rbias", [128, NE], F32)
            c_initB = S.chan("initB")
            c_initBp = S.chan("initBp")
            B_cB = Buf("constB")
            B_cBs = Buf("constBs")
            B_cBp = Buf("constBp")
            S.dma("pool", c_initBp, wout[:], wout_d.rearrange("(c p) n -> p c n", p=128), writes=[B_cBp], last=True)
            S.dma("sp", c_initB, ln1g[:], ln1g_d, writes=[B_cBs], last=False)
            S.dma("sp", c_initB, ln1b[:], ln1b_d, writes=[B_cBs], last=False)
            S.dma("sp", c_initB, ln2g[:], ln2g_d, writes=[B_cBs], last=False)
            S.dma("sp", c_initB, ln2b[:], ln2b_d, writes=[B_cBs], last=False)
            S.dma("sp", c_initB, wr[:], wr_d.rearrange("(c p) n -> p c n", p=128), writes=[B_cBs], last=False)
            S.dma("sp", c_initB, rbias[:], rbias_d, writes=[B_cBs], last=True)

            yacc = sbt(esB, "yacc", [128, TPB, D], F32)
            B_yacc = [Buf("yacc%d" % i) for i in range(TPB)]
            hTs = [sbt(esB, "hT%d" % i, [128, 8, tb], BF16) for i in range(2)]
            B_hTs = [[Buf("hT%d_%d" % (j, i)) for i in range(NSB)] for j in range(2)]
            gatess = [sbt(esB, "gates%d" % i, [128, TPB, NE + 1], F32) for i in range(2)]
            B_gatess = [[Buf("gates%d_%d" % (j, i)) for i in range(TPB)] for j in range(2)]
            S.op("dve", lambda e: e.memset(gatess[0][:], 1.0), reads=[B_cBs, B_cBp], writes=B_gatess[0] + [B_cB])
            S.op("dve", lambda e: e.memset(gatess[1][:], 1.0), writes=B_gatess[1])
            B_h1d = [Buf("h1d%d" % i) for i in range(NT // 128)]
            c_h1 = [S.chan("h1_%d" % i) for i in range(2)]
            c_ya = [S.chan("ya%d" % i) for i in range(2)]
            NWS = 3
            wgu_e = [sbt(esB, "wgu_e%d" % i, [128, 8, 512], BF16) for i in range(NWS)]
            wd_e = [sbt(esB, "wd_e%d" % i, [128, 2, D], BF16) for i in range(NWS)]
            B_we = [Buf("we%d" % i) for i in range(NWS)]
            c_we = [S.chan("we%d" % i) for i in range(NWS)]
            mts = [sbt(esB, "mts%d" % i, [128, 8, 128], BF16) for i in range(2)]
            B_mts = [Buf("mts%d" % i) for i in range(2)]
            c_mts = [S.chan("mts%d" % i) for i in range(2)]
            xs_ = [sbt(esB, "xs%d" % i, [128, D], F32) for i in range(2)]
            B_xs = [Buf("xs%d" % i) for i in range(2)]
            c_xs = [S.chan("xs%d" % i) for i in range(2)]
            ssum = [sbt(esB, "ssum%d" % i, [128, D], F32) for i in range(2)]
            B_ssum = [Buf("ssum%d" % i) for i in range(2)]
            hf = [sbt(esB, "hf%d" % i, [128, D], F32) for i in range(2)]
            B_hf = [Buf("hf%d" % i) for i in range(2)]
            hT32 = [sbt(esB, "hT32_%d" % i, [128, 8, 128], F32) for i in range(2)]
            B_hT32 = [Buf("hT32_%d" % i) for i in range(2)]
            stats = [sbt(esB, "stats%d" % i, [128, 2, 6], F32) for i in range(2)]
            mv = [sbt(esB, "mv%d" % i, [128, 2], F32) for i in range(2)]
            rs_ = [sbt(esB, "rs%d" % i, [128, 1], F32) for i in range(2)]
            B_stats = [Buf("stats%d" % i) for i in range(2)]
            sc = [sbt(esB, "sc%d" % i, [128, NE], F32) for i in range(2)]
            bia = [sbt(esB, "bia%d" % i, [128, NE], F32) for i in range(2)]
            srt = [sbt(esB, "srt%d" % i, [128, 8, 8], F32) for i in range(2)]
            gsc = [sbt(esB, "gsc%d" % i, [128, 8], F32) for i in range(2)]
            gtop = [sbt(esB, "gtop%d" % i, [128, 8], F32) for i in range(2)]
            gm = [sbt(esB, "gm%d" % i, [128, 8], F32) for i in range(2)]
            pen = [sbt(esB, "pen%d" % i, [128, 8], F32) for i in range(2)]
            msk = [sbt(esB, "msk%d" % i, [128, NE], F32) for i in range(2)]
            etop = [sbt(esB, "etop%d" % i, [128, 8], F32) for i in range(2)]
            sel = [sbt(esB, "sel%d" % i, [128, NE], F32) for i in range(2)]
            ssc = [sbt(esB, "ssc%d" % i, [128, 1], F32) for i in range(2)]
            B_rt = [Buf("rt%d" % i) for i in range(2)]
            sgs = [sbt(esB, "sgs%d" % i, [128, 512], F32) for i in range(2)]
            B_sgs = [Buf("sgs%d" % i) for i in range(2)]
            hh = [sbt(esB, "hh%d" % i, [128, 2, 512], BF16) for i in range(2)]
            B_hh = [Buf("hh%d" % i) for i in range(2)]
            ob = [sbt(esB, "ob%d" % i, [128, D], F32) for i in range(2)]
            B_ob = [Buf("ob%d" % i) for i in range(2)]
            c_ob = [S.chan("ob%d" % i) for i in range(2)]
            B_outd = Buf("out_dram")

            def layer_norm(p2, src_ap, srcbufs, g_t, b_t, dst_ap, dstbufs, tmp_ap, tmpbuf):
                for c in range(2):
                    S.op("dve", (lambda c: lambda e: e.bn_stats(out=stats[p2][:, c, :], in_=src_ap[:, c * 512:(c + 1) * 512]))(c),
                         reads=srcbufs, writes=[B_stats[p2]])
                S.op("dve", lambda e: e.bn_aggr(out=mv[p2][:], in_=stats[p2][:].rearrange("p a b -> p (a b)")), reads=[B_stats[p2]], writes=[B_stats[p2]])
                S.op("act", lambda e: e.activation(out=rs_[p2][:], in_=mv[p2][:, 1:2], func=AF.Ln, bias=float(LN_EPS)),
                     reads=[B_stats[p2]], writes=[B_stats[p2]])
                S.op("act", lambda e: e.activation(out=rs_[p2][:], in_=rs_[p2][:], func=AF.Exp, scale=-0.5),
                     reads=[B_stats[p2]], writes=[B_stats[p2]])
                S.op("dve", lambda e: e.tensor_scalar(out=tmp_ap, in0=src_ap, scalar1=mv[p2][:, 0:1], scalar2=rs_[p2][:, 0:1],
                                                      op0=ALU.subtract, op1=ALU.mult), reads=list(srcbufs) + [B_stats[p2]], writes=[tmpbuf])
                S.op("pool", lambda e: e.tensor_tensor(out=tmp_ap, in0=tmp_ap, in1=g_t[:], op=ALU.mult), reads=[tmpbuf, B_cB], writes=[tmpbuf])
                S.op("pool", lambda e: e.tensor_tensor(out=dst_ap, in0=tmp_ap, in1=b_t[:], op=ALU.add), reads=[tmpbuf, B_cB], writes=dstbufs)

            def load_w(e, slot):
                S.dma("pool", c_we[slot], wgu_e[slot][:, :, 0:256], weg_d[e].rearrange("(c p) f -> p c f", p=128), writes=[B_we[slot]], last=False)
                S.dma("pool", c_we[slot], wgu_e[slot][:, :, 256:512], weu_d[e].rearrange("(c p) f -> p c f", p=128), writes=[B_we[slot]], last=False)
                S.dma("pool", c_we[slot], wd_e[slot][:], wed_d[e].rearrange("(f p) d -> p f d", p=128), writes=[B_we[slot]], last=True)

            wcount = {"n": 0}
            NEXP = NE + 1
            total_w = NTB * NEXP

            def issue_w(n):
                if n < total_w:
                    load_w(n % NEXP, n % NWS)

            if stop == 'B0':
                S.barrier()
                return nc
            issue_w(0)
            issue_w(1)
            gt = {"n": 0}
            ppool = {"p": (0, 8), "y": (4, 8)}

            def prologue_tile(tbi, tl, phase):
                if True:
                    hT = hTs[tbi % 2]
                    B_hT = B_hTs[tbi % 2]
                    gates = gatess[tbi % 2]
                    B_gates = B_gatess[tbi % 2]
                    gti = tbi * TPB + tl
                    p2 = gti % 2
                    sbi = tl // 4
                    if phase == 0:
                        S.dma("sp", c_mts[p2], mts[p2][:], mT_d[:, gti, :, :], writes=[B_mts[p2]])
                        S.dma("sp", c_xs[p2], xs_[p2][:], x_d[gti * 128:(gti + 1) * 128, :], writes=[B_xs[p2]])
                        return
                    if phase == 1:
                        prologue_p1(gti, p2)
                        return
                    prologue_p2(hT, B_hT, gates, B_gates, tl, p2, sbi)

            def prologue_p1(gti, p2):
                if True:
                    for half in range(2):
                        pm, bm = ps_next(*ppool["p"])
                        S.mm([(lambda dc, half: lambda pe: pe.matmul(pm[:, :], lhsT=mts[p2][:, dc, :], rhs=wout[:, dc, half * 512:(half + 1) * 512],
                                                                      start=(dc == 0), stop=(dc == 7)))(dc, half) for dc in range(8)],
                             reads=[B_mts[p2], B_cB], writes=[bm])
                        S.op("dve", (lambda half, pm: lambda e: e.scalar_tensor_tensor(
                            out=ssum[p2][:, half * 512:(half + 1) * 512], in0=xs_[p2][:, half * 512:(half + 1) * 512], scalar=ALPHA, in1=pm[:, :],
                            op0=ALU.mult, op1=ALU.add))(half, pm), reads=[B_xs[p2], bm], writes=[B_ssum[p2]])
                    layer_norm(p2, ssum[p2][:], [B_ssum[p2]], ln1g, ln1b, hf[p2][:], [B_hf[p2]], ssum[p2][:], B_ssum[p2])
                    if dbg:
                        S.dma("sp", c_dbg[p2], dbg_d["h1"][gti * 128:(gti + 1) * 128, :], hf[p2][:], reads=[B_hf[p2]])
                    S.op("act", lambda e: e.activation(out=ssum[p2][:], in_=hf[p2][:], func=AF.Copy, scale=ALPHA), reads=[B_hf[p2]], writes=[B_ssum[p2]])
                    S.dma("sp", c_h1[p2], h1_d[gti * 128:(gti + 1) * 128, :], ssum[p2][:], reads=[B_ssum[p2]], writes=[B_h1d[gti]])

            def prologue_p2(hT, B_hT, gates, B_gates, tl, p2, sbi):
                if True:
                    for half in range(2):
                        ptr, btr = ps_next(*ppool["p"])
                        for cc in range(4):
                            c = half * 4 + cc
                            S.mm([(lambda c, cc: lambda pe: pe.matmul(ptr[:, cc * 128:(cc + 1) * 128], lhsT=hf[p2][:, c * 128:(c + 1) * 128], rhs=ident[:, :], start=True, stop=True))(c, cc)],
                                 reads=[B_hf[p2], B_const], writes=[btr])
                        p3 = ptr[:, :].rearrange("p (c t) -> p c t", c=4)
                        S.op("act", (lambda half, p3: lambda e: e.copy(out=hT[:, half * 4:(half + 1) * 4, tl * 128:(tl + 1) * 128], in_=p3))(half, p3),
                             reads=[btr], writes=[B_hT[sbi]])
                        S.op("dve", (lambda half, p3: lambda e: e.tensor_copy(out=hT32[p2][:, half * 4:(half + 1) * 4, :], in_=p3))(half, p3),
                             reads=[btr], writes=[B_hT32[p2]])
                    plg, blg = ps_next(*ppool["p"])
                    S.mm([(lambda c: lambda pe: pe.matmul(plg[:, 0:NE], lhsT=hT32[p2][:, c, :], rhs=wr[:, c, :], start=(c == 0), stop=(c == 7)))(c)
                          for c in range(8)], reads=[B_hT32[p2], B_cB], writes=[blg])
                    R = [B_rt[p2]]
                    S.op("act", lambda e: e.activation(out=sc[p2][:], in_=plg[:, 0:NE], func=AF.Sigmoid), reads=[blg], writes=R)
                    S.op("dve", lambda e: e.tensor_tensor(out=bia[p2][:], in0=sc[p2][:], in1=rbias[:], op=ALU.add), reads=R + [B_cB], writes=R)
                    for g in range(8):
                        S.op("dve", (lambda g: lambda e: e.max(out=srt[p2][:, g, :], in_=bia[p2][:, g * 8:(g + 1) * 8]))(g), reads=R, writes=R)
                    S.op("dve", lambda e: e.tensor_tensor(out=gsc[p2][:], in0=srt[p2][:, :, 0], in1=srt[p2][:, :, 1], op=ALU.add), reads=R, writes=R)
                    S.op("dve", lambda e: e.max(out=gtop[p2][:], in_=gsc[p2][:]), reads=R, writes=R)
                    S.op("dve", lambda e: e.tensor_scalar(out=gm[p2][:], in0=gsc[p2][:], scalar1=gtop[p2][:, 3:4], scalar2=None, op0=ALU.is_ge), reads=R, writes=R)
                    S.op("dve", lambda e: e.tensor_scalar(out=pen[p2][:], in0=gm[p2][:], scalar1=-1.0, scalar2=1e30, op0=ALU.add, op1=ALU.mult), reads=R, writes=R)
                    for g in range(8):
                        S.op("dve", (lambda g: lambda e: e.tensor_scalar(out=msk[p2][:, g * 8:(g + 1) * 8], in0=bia[p2][:, g * 8:(g + 1) * 8],
                                                                         scalar1=gm[p2][:, g:g + 1], scalar2=pen[p2][:, g:g + 1],
                                                                         op0=ALU.mult, op1=ALU.add))(g), reads=R, writes=R)
                    S.op("dve", lambda e: e.max(out=etop[p2][:], in_=msk[p2][:]), reads=R, writes=R)
                    S.op("dve", lambda e: e.tensor_scalar(out=sel[p2][:], in0=msk[p2][:], scalar1=etop[p2][:, 7:8], scalar2=None, op0=ALU.is_ge), reads=R, writes=R)
                    S.op("dve", lambda e: e.tensor_tensor(out=sel[p2][:], in0=sel[p2][:], in1=sc[p2][:], op=ALU.mult), reads=R, writes=R)
                    S.op("dve", lambda e: e.reduce_sum(out=ssc[p2][:], in_=sel[p2][:], axis=AX.X), reads=R, writes=R)
                    S.op("dve", lambda e: e.reciprocal(out=ssc[p2][:], in_=ssc[p2][:]), reads=R, writes=R)
                    S.op("dve", lambda e: e.tensor_scalar(out=gates[:, tl, 0:NE], in0=sel[p2][:], scalar1=ssc[p2][:, 0:1], scalar2=2.5,
                                                          op0=ALU.mult, op1=ALU.mult), reads=R, writes=[B_gates[tl]])
            prologue_tile(0, 0, 0)
            for tl in range(TPB + 1):
                if tl + 1 < TPB:
                    prologue_tile(0, tl + 1, 0)
                if tl < TPB:
                    prologue_tile(0, tl, 1)
                if tl >= 1:
                    prologue_tile(0, tl - 1, 2)
            for tbi in range(NTB):
                hT = hTs[tbi % 2]
                B_hT = B_hTs[tbi % 2]
                gates = gatess[tbi % 2]
                B_gates = B_gatess[tbi % 2]
                if dbg:
                    S.dma("sp", c_dbg[3], dbg_d["gates"][:, tbi * TPB:(tbi + 1) * TPB, :], gates[:], reads=B_gates)
                if stop == 'B1':
                    S.barrier()
                    return nc
                for tl in range(TPB):
                    gti = tbi * TPB + tl
                    S.dma("sp", c_ya[(tl // 4) % 2], yacc[:, tl, :], h1_d[gti * 128:(gti + 1) * 128, :], reads=[B_h1d[gti]], writes=[B_yacc[tl]], last=(tl % 4 == 3 or tl == TPB - 1))
                units = [(e, sbi) for e in range(NEXP) for sbi in range(NSB)]
                pro_every = max(1, len(units) // TPB)
                if tbi + 1 < NTB:
                    ppool["p"], ppool["y"] = (4, 8), (4, 8)
                else:
                    ppool["p"], ppool["y"] = (0, 8), (4, 8)

                def emit_gu(u, fcs):
                    e, sbi = units[u]
                    n = tbi * NEXP + e
                    slot = n % NWS
                    h2 = u % 2
                    for fc in fcs:
                        pgp, bgp = ps_next(0, 4)
                        S.mm([(lambda c, fc: lambda pe: pe.matmul(pgp[:, :], lhsT=wgu_e[slot][:, c, fc * 128:(fc + 1) * 128],
                                                                   rhs=hT[:, c, sbi * 512:(sbi + 1) * 512], start=(c == 0), stop=(c == 7)))(c, fc) for c in range(8)],
                             reads=[B_we[slot], B_hT[sbi]], writes=[bgp])
                        pup, bup = ps_next(0, 4)
                        S.mm([(lambda c, fc: lambda pe: pe.matmul(pup[:, :], lhsT=wgu_e[slot][:, c, 256 + fc * 128:256 + (fc + 1) * 128],
                                                                   rhs=hT[:, c, sbi * 512:(sbi + 1) * 512], start=(c == 0), stop=(c == 7)))(c, fc) for c in range(8)],
                             reads=[B_we[slot], B_hT[sbi]], writes=[bup])
                        s2 = fc
                        S.op("act", (lambda pgp, s2: lambda e_: e_.activation(out=sgs[s2][:], in_=pgp[:, :], func=AF.Silu))(pgp, s2), reads=[bgp], writes=[B_sgs[s2]])
                        S.op("dve", (lambda pup, s2, fc: lambda e_: e_.tensor_tensor(out=hh[h2][:, fc, :], in0=pup[:, :], in1=sgs[s2][:], op=ALU.mult))(pup, s2, fc),
                             reads=[bup, B_sgs[s2]], writes=[B_hh[h2]])

                def emit_down(u, tts):
                    e, sbi = units[u]
                    n = tbi * NEXP + e
                    slot = n % NWS
                    h2 = u % 2
                    S.prewait("pe", psbuf[4:8])
                    for tt in tts:
                        tl = sbi * 4 + tt
                        pys = [ps_next(*ppool["y"]) for _ in range(2)]
                        S.mm([(lambda fc, tt, dh, py: lambda pe: pe.matmul(py[:, :], lhsT=hh[h2][:, fc, tt * 128:(tt + 1) * 128],
                                                                            rhs=wd_e[slot][:, fc, dh * 512:(dh + 1) * 512], start=(fc == 0), stop=(fc == 1)))(fc, tt, dh, pys[dh][0])
                              for dh in range(2) for fc in range(2)], reads=[B_hh[h2], B_we[slot]], writes=[pys[0][1], pys[1][1]])
                        for dh in range(2):
                            py, by = pys[dh]
                            S.op("dve", (lambda py, tl, dh, e: lambda e_: e_.scalar_tensor_tensor(
                                out=yacc[:, tl, dh * 512:(dh + 1) * 512], in0=py[:, :], scalar=gates[:, tl, e:e + 1], in1=yacc[:, tl, dh * 512:(dh + 1) * 512],
                                op0=ALU.mult, op1=ALU.add))(py, tl, dh, e), reads=[by, B_gates[tl], B_yacc[tl]], writes=[B_yacc[tl]])

                for u in range(len(units)):
                    e, sbi = units[u]
                    emit_gu(u, (0,))
                    if u > 0:
                        emit_down(u - 1, (0, 1))
                    emit_gu(u, (1,))
                    if u > 0:
                        emit_down(u - 1, (2, 3))
                    if sbi == 0:
                        issue_w(tbi * NEXP + e + 2)
                    if tbi + 1 < NTB and u // pro_every < TPB:
                        ph = {1: 0, pro_every // 4 + 1: 1, (3 * pro_every) // 4: 2}.get(u % pro_every)
                        if ph is not None:
                            prologue_tile(tbi + 1, u // pro_every, ph)
                emit_down(len(units) - 1, (0, 1, 2, 3))
                if stop == 'B2':
                    S.barrier()
                    return nc
                for tl in range(TPB):
                    gti = tbi * TPB + tl
                    p2 = gti % 2
                    layer_norm(p2, yacc[:, tl, :], [B_yacc[tl]], ln2g, ln2b, ob[p2][:], [B_ob[p2]], ob[p2][:], B_ob[p2])
                    S.dma("sp", c_ob[p2], out_d[gti * 128:(gti + 1) * 128, :], ob[p2][:], reads=[B_ob[p2]], writes=[B_outd])
            S.wait_all_on("sp")
            S.barrier()
        print("instrs", S.ninstr, "waits", S.nwaits, flush=True)
    return nc


def _bands(first_is_seq_start):
    cur = np.zeros((128, 4, 128), np.float32)
    prev = np.zeros((128, 4, 128), np.float32)
    first = np.zeros((128, 4, 128), np.float32)
    s = np.arange(128)[:, None]
    t = np.arange(128)[None, :]
    for g, w in enumerate(POOL_W):
        inwin = ((s <= t) & (s > t - w)).astype(np.float32)
        cur[:, g, :] = inwin / w - (s == t)
        prev[:, g, :] = ((s - 128) > (t - w)).astype(np.float32) / w
        cnt = np.minimum(t + 1, w).astype(np.float32)
        first[:, g, :] = inwin / cnt - (s == t)
    if not first_is_seq_start:
        first = cur.copy()
    return cur, prev, first


def _prep_inputs(inp):
    x = np.asarray(inp["x"], np.float32)
    B, SEQ, _ = x.shape
    NSEG = 8 // B
    NT = SEQ // NSEG
    NPREV = NT * (NSEG - 1)
    f = lambda k: np.ascontiguousarray(np.asarray(inp[k], np.float32)[0])
    shared = {}
    shared["w_in"] = f("w_in")
    wgu17 = np.zeros((32, 512), np.float32)
    wgu17[0:16] = f("w_gate_up")
    wgu17[16] = f("b_gate")
    shared["wgu17"] = wgu17
    shared["gng"] = np.ascontiguousarray(f("gla_norm_g").reshape(2, 128).T)
    shared["w_gla_up"] = f("w_gla_up")
    shared["w_pool_grp"] = f("w_pool_grp")
    shared["pscale"] = np.ascontiguousarray(f("pool_scale").reshape(4, 128).T)
    shared["w_pool_up"] = f("w_pool_up")
    shared["w_out"] = f("w_out")
    for k_, n_ in (("ln1_g", "ln1g"), ("ln1_b", "ln1b"), ("ln2_g", "ln2g"), ("ln2_b", "ln2b")):
        shared[n_] = np.ascontiguousarray(np.broadcast_to(f(k_)[None, :], (128, D)))
    shared["w_router"] = f("w_router")
    shared["rbias"] = np.ascontiguousarray(np.broadcast_to(f("router_bias")[None, :], (128, NE)))
    shared["w_eg"] = np.concatenate([f("w_exp_gate"), f("w_sh_gate")[None]], 0)
    shared["w_eu"] = np.concatenate([f("w_exp_up"), f("w_sh_up")[None]], 0)
    shared["w_ed"] = np.concatenate([f("w_exp_down"), f("w_sh_down")[None]], 0)
    m = np.arange(128)[:, None]
    i = np.arange(128)[None, :]
    shared["tri_u"] = np.where(m <= i, -1.0 / 16.0, 0.0).astype(np.float32)
    shared["tri_r"] = np.where(m > i, -1.0 / 16.0, 0.0).astype(np.float32)
    mk = (m <= i).astype(np.float32)
    shared["mask4"] = np.ascontiguousarray(np.broadcast_to(mk[:, None, :], (128, 4, 128)))
    shared["ident"] = np.eye(128, dtype=np.float32)
    shared["ones"] = np.ones((128, 128), np.float32)
    in_maps = []
    for core in range(8):
        b, sgm = core // NSEG, core % NSEG
        xs = x[b, sgm * NT:(sgm + 1) * NT]
        mp = dict(shared)
        mp["x"] = np.ascontiguousarray(xs)
        mp["xT"] = np.ascontiguousarray(xs.T.reshape(8, 128, NT))
        xp = np.zeros((NPREV, D), np.float32)
        if sgm > 0:
            xp[NPREV - sgm * NT:] = x[b, 0:sgm * NT]
        mp["xpT"] = np.ascontiguousarray(xp.T.reshape(8, 128, NPREV))
        cur, prev, first = _bands(sgm == 0)
        mp["band_cur"], mp["band_prev"], mp["band_first"] = cur, prev, first
        in_maps.append(mp)
    return in_maps, B, SEQ, NT, NPREV


def kernel(**inputs):
    in_maps, B, SEQ, NT, NPREV = _prep_inputs(inputs)
    nc = build_program(NT, NPREV)
    res = run_bass_kernel_spmd(nc, in_maps, core_ids=list(range(8)))
    out = np.empty((B, SEQ, D), np.float32)
    NSEG = 8 // B
    for core in range(8):
        b, sgm = core // NSEG, core % NSEG
        out[b, sgm * NT:(sgm + 1) * NT] = res.results[core]["out"]
    return out
```
